# Optimizing a Trainium2 kernel written in Bass

```python
import jax
import jax.numpy as jnp
from jax import lax
import numpy as np

D_MODEL = 2048
BATCH = 32
SEQ = 256
DEPTH = 2
DEC_BATCH = 2
DEC_SEQ = 4096
PAST_LEN = 256

GRID_W = 64
WA = D_MODEL // 2
WB = D_MODEL // 4
WC = D_MODEL - WA - WB
HEAD_DIM_A = 64
N_HEADS_A = WA // HEAD_DIM_A
HEAD_DIM_B = 64
N_HEADS_B = WB // HEAD_DIM_B
POOL_WINDOWS = (2, 4, 8, 16)
N_POOL_GROUPS = len(POOL_WINDOWS)
POOL_GROUP_DIM = WC // N_POOL_GROUPS
NA_ROWS = 8
NA_COLS = 16
Q_BLOCK = 128
DECAY_LORA = max(32, int(round(1.8 * D_MODEL ** 0.5 / 32)) * 32)
AAA_LORA = max(32, int(round(1.8 * D_MODEL ** 0.5 / 32)) * 32)
GATE_LORA = max(32, int(round(0.6 * D_MODEL ** 0.8 / 32)) * 32)
CONV_W = 3
OFF_Q = 0
OFF_K = WA
OFF_V = 2 * WA
OFF_RKV = 3 * WA
OFF_WL = OFF_RKV + 3 * WB
OFF_AL = OFF_WL + 2 * DECAY_LORA
OFF_GL = OFF_AL + 2 * AAA_LORA
OFF_POOL = OFF_GL + GATE_LORA
P_IN = OFF_POOL + WC
D_FF = ((8 * D_MODEL // 3 + 255) // 256) * 256
N_EXPERTS = 8
TOP_K = 2
D_FF_EXPERT = 7 * D_MODEL // 2
N_DENSE = (DEPTH + 1) // 2
N_MOE = DEPTH // 2
RMS_EPS = 1e-6
GN_EPS = 64e-5
NEG_INF = -1e30

kernel_name = "hybrid_dit_prefix_natten_rwkv7_pool_step"


def rms_norm(x, g):
    xf = x.astype(jnp.float32)
    y = xf * lax.rsqrt(jnp.mean(xf * xf, axis=-1, keepdims=True) + RMS_EPS)
    return (y * g.astype(jnp.float32)).astype(x.dtype)


def ada_mod(cond, w, b):
    m = (jax.nn.silu(cond) @ w + b)[..., None, :]
    return jnp.split(m, 6, axis=-1)


def modulate(x, g, shift, scale):
    return rms_norm(x, g) * (1.0 + scale) + shift


def short_conv(x, w):
    T = x.shape[1]
    pad = CONV_W // 2
    xp = jnp.pad(x, ((0, 0), (pad, CONV_W - 1 - pad), (0, 0)))
    y = xp[:, 0:T] * w[0]
    for i in range(1, CONV_W):
        y = y + xp[:, i:i + T] * w[i]
    return y


def attn_context(q, k, v):
    B, S, _ = q.shape
    qh = q.reshape(B, S // Q_BLOCK, Q_BLOCK, N_HEADS_A, HEAD_DIM_A)
    kh = k.reshape(B, S, N_HEADS_A, HEAD_DIM_A)
    vh = v.reshape(B, S, N_HEADS_A, HEAD_DIM_A)

    def blk(qb):
        s = jnp.einsum('bqhd,blhd->bhql', qb, kh, preferred_element_type=jnp.float32) * HEAD_DIM_A ** -0.5
        p = jax.nn.softmax(s, axis=-1).astype(vh.dtype)
        return jnp.einsum('bhql,blhd->bqhd', p, vh)

    o = lax.map(blk, jnp.moveaxis(qh, 1, 0))
    o = jnp.moveaxis(o, 0, 1).reshape(B, S, WA)
    return o, kh, vh


def neighbourhood_attention(q, k, v, ck, cv, rpb):
    B, T, _ = q.shape
    rows = T // GRID_W
    kr = min(NA_ROWS, rows)
    qg = q.reshape(B, rows, GRID_W, N_HEADS_A, HEAD_DIM_A)
    kg = k.reshape(B, rows, GRID_W, N_HEADS_A, HEAD_DIM_A)
    vg = v.reshape(B, rows, GRID_W, N_HEADS_A, HEAD_DIM_A)
    r = jnp.arange(rows)
    rs = jnp.clip(r - kr // 2, 0, rows - kr)
    ridx = rs[:, None] + jnp.arange(kr)[None, :]
    kb = kg[:, ridx]
    vb = vg[:, ridx]
    scale = HEAD_DIM_A ** -0.5
    s_loc = jnp.einsum('brchd,brkmhd->bhrckm', qg, kb, preferred_element_type=jnp.float32) * scale
    col = jnp.arange(GRID_W)
    cs = jnp.clip(col - NA_COLS // 2, 0, GRID_W - NA_COLS)
    valid = (col[None, :] >= cs[:, None]) & (col[None, :] < cs[:, None] + NA_COLS)
    dci = jnp.clip(col[None, :] - col[:, None] + NA_COLS - 1, 0, 2 * NA_COLS - 2)
    dri = ridx - r[:, None] + NA_ROWS - 1
    bias = rpb.astype(jnp.float32)[:, dri[:, None, :, None], dci[None, :, None, :]]
    s_loc = jnp.where(valid[:, None, :], s_loc + bias, NEG_INF)
    n_loc = kr * GRID_W
    s_loc = s_loc.reshape(B, N_HEADS_A, rows, GRID_W, n_loc)
    s_ctx = jnp.einsum('brchd,blhd->bhrcl', qg, ck, preferred_element_type=jnp.float32) * scale
    p = jax.nn.softmax(jnp.concatenate([s_loc, s_ctx], axis=-1), axis=-1)
    p_loc = p[..., :n_loc].reshape(B, N_HEADS_A, rows, GRID_W, kr, GRID_W).astype(v.dtype)
    p_ctx = p[..., n_loc:].astype(v.dtype)
    o = jnp.einsum('bhrckm,brkmhd->brchd', p_loc, vb) + jnp.einsum('bhrcl,blhd->brchd', p_ctx, cv.astype(v.dtype))
    return o.reshape(B, T, WA)


def rwkv_scan(s0, r, decay, kk, bvec, kd, v, reverse):
    B, T, _ = r.shape

    def heads(z):
        return jnp.moveaxis(z.astype(jnp.float32).reshape(B, T, N_HEADS_B, HEAD_DIM_B), 1, 0)

    def step(S, inp):
        r_t, w_t, kk_t, b_t, k_t, v_t = inp
        sa = jnp.einsum('bhij,bhj->bhi', S, -kk_t)
        S = S * w_t[:, :, None, :] + sa[..., None] * b_t[:, :, None, :] + v_t[..., None] * k_t[:, :, None, :]
        return S, jnp.einsum('bhij,bhj->bhi', S, r_t)

    xs = (heads(r), heads(decay), heads(kk), heads(bvec), heads(kd), heads(v))
    s_fin, o = lax.scan(step, s0.astype(jnp.float32), xs, reverse=reverse)
    return jnp.moveaxis(o, 0, 1), s_fin


def rwkv_mix(p, s_fwd0, s_bwd0, conv_w, w0, w2, a0, a2, g2, k_k, k_a, r_k, ln_g, ln_b):
    B, T, _ = p.shape
    f32 = jnp.float32
    rkv = short_conv(p[..., OFF_RKV:OFF_WL], conv_w).astype(f32)
    r, k, v = jnp.split(rkv, 3, axis=-1)
    xw = p[..., OFF_WL:OFF_AL].astype(f32).reshape(B, T, 2, DECAY_LORA)
    xa = p[..., OFF_AL:OFF_GL].astype(f32).reshape(B, T, 2, AAA_LORA)
    xg = p[..., OFF_GL:OFF_POOL].astype(f32)
    wl = w0.astype(f32) + jnp.einsum('btdr,drc->btdc', jnp.tanh(xw), w2.astype(f32))
    decay = jnp.exp(-jnp.exp(-jax.nn.softplus(-wl) - 0.5))
    a = jax.nn.sigmoid(a0.astype(f32) + jnp.einsum('btdr,drc->btdc', xa, a2.astype(f32)))
    g = jax.nn.sigmoid(xg) @ g2.astype(f32)
    kk = (k * k_k.astype(f32)).reshape(B, T, N_HEADS_B, HEAD_DIM_B)
    kk = (kk * lax.rsqrt(jnp.sum(kk * kk, axis=-1, keepdims=True) + 1e-12)).reshape(B, T, WB)
    kd = k[:, :, None, :] * (1.0 + (a - 1.0) * k_a.astype(f32))
    bvec = kk[:, :, None, :] * a
    o_f, s_f = rwkv_scan(s_fwd0, r, decay[:, :, 0], kk, bvec[:, :, 0], kd[:, :, 0], v, False)
    o_b, s_b = rwkv_scan(s_bwd0, r, decay[:, :, 1], kk, bvec[:, :, 1], kd[:, :, 1], v, True)
    o = o_f + o_b
    mu = jnp.mean(o, axis=-1, keepdims=True)
    var = jnp.mean(jnp.square(o - mu), axis=-1, keepdims=True)
    o = ((o - mu) * lax.rsqrt(var + GN_EPS)).reshape(B, T, WB) * ln_g.astype(f32) + ln_b.astype(f32)
    bonus = jnp.sum((r * k * r_k.astype(f32)).reshape(B, T, N_HEADS_B, HEAD_DIM_B), axis=-1, keepdims=True) \
        * v.reshape(B, T, N_HEADS_B, HEAD_DIM_B)
    o = (o + bonus.reshape(B, T, WB)) * g
    return o.astype(p.dtype), s_f, s_b


def multiscale_pool(u, pool_w, pool_scale):
    B, T, _ = u.shape
    uf = u.astype(jnp.float32).reshape(B, T, N_POOL_GROUPS, POOL_GROUP_DIM)
    csum = jnp.concatenate([jnp.zeros((B, 1, N_POOL_GROUPS, POOL_GROUP_DIM), jnp.float32),
                            jnp.cumsum(uf, axis=1)], axis=1)
    t = jnp.arange(T)
    outs = []
    for gi, win in enumerate(POOL_WINDOWS):
        lo = jnp.maximum(t - win // 2, 0)
        hi = jnp.minimum(t + win - win // 2, T)
        cg = csum[:, :, gi]
        cnt = (hi - lo).astype(jnp.float32)[None, :, None]
        outs.append((cg[:, hi] - cg[:, lo]) / cnt - uf[:, :, gi])
    pooled = jnp.stack(outs, axis=2)
    y = jnp.einsum('btgc,gcd->btgd', pooled, pool_w.astype(jnp.float32)).reshape(B, T, WC)
    return (y * pool_scale.astype(jnp.float32)).astype(u.dtype)


def mixers_context(h, w_in, w_out, rw, pool_w, pool_scale):
    p = h @ w_in
    B = h.shape[0]
    oa, kh, vh = attn_context(p[..., OFF_Q:OFF_K], p[..., OFF_K:OFF_V], p[..., OFF_V:OFF_RKV])
    s0 = jnp.zeros((B, N_HEADS_B, HEAD_DIM_B, HEAD_DIM_B), jnp.float32)
    ob, s_f, s_b = rwkv_mix(p, s0, s0, *rw)
    oc = multiscale_pool(p[..., OFF_POOL:P_IN], pool_w, pool_scale)
    y = jnp.concatenate([oa, ob, oc], axis=-1) @ w_out
    return y, kh, vh, s_f, s_b


def mixers_latent(h, ck, cv, s_f0, s_b0, w_in, w_out, rpb, rw, pool_w, pool_scale):
    p = h @ w_in
    oa = neighbourhood_attention(p[..., OFF_Q:OFF_K], p[..., OFF_K:OFF_V], p[..., OFF_V:OFF_RKV], ck, cv, rpb)
    ob, _, _ = rwkv_mix(p, s_f0, s_b0, *rw)
    oc = multiscale_pool(p[..., OFF_POOL:P_IN], pool_w, pool_scale)
    return jnp.concatenate([oa, ob, oc], axis=-1) @ w_out


def swiglu(h, w1, w3, w2):
    return (jax.nn.silu(h @ w1) * (h @ w3)) @ w2


def moe_swiglu(h, router, w1, w3, w2):
    logits = (h @ router).astype(jnp.float32)
    top_v, top_i = lax.top_k(logits, TOP_K)
    top_p = jax.nn.softmax(top_v, axis=-1)
    gates = jnp.sum(jax.nn.one_hot(top_i, N_EXPERTS, dtype=jnp.float32) * top_p[..., None], axis=-2)
    gates = gates.astype(h.dtype)
    y = gates[..., 0:1] * swiglu(h, w1[0], w3[0], w2[0])
    for e in range(1, N_EXPERTS):
        y = y + gates[..., e:e + 1] * swiglu(h, w1[e], w3[e], w2[e])
    return y


def setup_inputs(seed: int = 0) -> dict:
    key = jax.random.key(seed)
    ks = iter(jax.random.split(key, 48))

    def nrm(shape, s=1.0):
        return jax.random.normal(next(ks), shape, jnp.float32) * s

    d = D_MODEL
    centre = (jnp.arange(CONV_W) == CONV_W // 2).astype(jnp.float32)[None, :, None]
    return {
        "x_prompt": nrm((BATCH, SEQ, d)),
        "x_sample": nrm((DEC_BATCH, DEC_SEQ, d)),
        "cache_attn_k": nrm((DEC_BATCH, DEPTH, PAST_LEN, N_HEADS_A, HEAD_DIM_A)),
        "cache_attn_v": nrm((DEC_BATCH, DEPTH, PAST_LEN, N_HEADS_A, HEAD_DIM_A)),
        "state_rwkv_fwd": nrm((DEC_BATCH, DEPTH, N_HEADS_B, HEAD_DIM_B, HEAD_DIM_B), 0.5),
        "state_rwkv_bwd": nrm((DEC_BATCH, DEPTH, N_HEADS_B, HEAD_DIM_B, HEAD_DIM_B), 0.5),
        "c": nrm((DEC_BATCH, d)),
        "c_ctx": nrm((d,)),
        "norm1_g": 1.0 + nrm((DEPTH, d), 0.02),
        "norm2_g": 1.0 + nrm((DEPTH, d), 0.02),
        "ada_w": nrm((DEPTH, d, 6 * d), 0.5 * d ** -0.5),
        "ada_b": nrm((DEPTH, 6 * d), 0.02),
        "w_in": nrm((DEPTH, d, P_IN), d ** -0.5),
        "w_out": nrm((DEPTH, d, d), d ** -0.5),
        "na_rpb": nrm((DEPTH, N_HEADS_A, 2 * NA_ROWS - 1, 2 * NA_COLS - 1), 0.1),
        "rw_conv": centre + nrm((DEPTH, CONV_W, 3 * WB), 0.2),
        "rw_w0": -1.5 + nrm((DEPTH, 2, WB), 0.5),
        "rw_w2": nrm((DEPTH, 2, DECAY_LORA, WB), 0.5 * DECAY_LORA ** -0.5),
        "rw_a0": nrm((DEPTH, 2, WB), 0.5),
        "rw_a2": nrm((DEPTH, 2, AAA_LORA, WB), AAA_LORA ** -0.5),
        "rw_g2": nrm((DEPTH, GATE_LORA, WB), GATE_LORA ** -0.5),
        "rw_kk": 0.85 + nrm((DEPTH, WB), 0.05),
        "rw_ka": 1.0 + nrm((DEPTH, WB), 0.05),
        "rw_rk": nrm((DEPTH, WB), 0.1),
        "rw_ln_g": 1.0 + nrm((DEPTH, WB), 0.02),
        "rw_ln_b": nrm((DEPTH, WB), 0.02),
        "pool_w": nrm((DEPTH, N_POOL_GROUPS, POOL_GROUP_DIM, POOL_GROUP_DIM), POOL_GROUP_DIM ** -0.5),
        "pool_scale": 1.0 + nrm((DEPTH, WC), 0.1),
        "ffn_w1": nrm((N_DENSE, d, D_FF), d ** -0.5),
        "ffn_w3": nrm((N_DENSE, d, D_FF), d ** -0.5),
        "ffn_w2": nrm((N_DENSE, D_FF, d), D_FF ** -0.5),
        "moe_router": nrm((N_MOE, d, N_EXPERTS), d ** -0.5),
        "moe_w1": nrm((N_MOE, N_EXPERTS, d, D_FF_EXPERT), d ** -0.5),
        "moe_w3": nrm((N_MOE, N_EXPERTS, d, D_FF_EXPERT), d ** -0.5),
        "moe_w2": nrm((N_MOE, N_EXPERTS, D_FF_EXPERT, d), D_FF_EXPERT ** -0.5),
        "final_g": 1.0 + nrm((d,), 0.02),
    }


def reference(x_prompt, x_sample, cache_attn_k, cache_attn_v, state_rwkv_fwd, state_rwkv_bwd, c, c_ctx,
              norm1_g, norm2_g, ada_w, ada_b, w_in, w_out, na_rpb, rw_conv, rw_w0, rw_w2, rw_a0, rw_a2,
              rw_g2, rw_kk, rw_ka, rw_rk, rw_ln_g, rw_ln_b, pool_w, pool_scale, ffn_w1, ffn_w3, ffn_w2,
              moe_router, moe_w1, moe_w3, moe_w2, final_g):
    xp = x_prompt
    xs = x_sample
    ks_new, vs_new, sf_new, sb_new = [], [], [], []
    for l in range(DEPTH):
        rw = (rw_conv[l], rw_w0[l], rw_w2[l], rw_a0[l], rw_a2[l], rw_g2[l], rw_kk[l], rw_ka[l], rw_rk[l],
              rw_ln_g[l], rw_ln_b[l])
        i = l // 2
        sh1, sc1, gt1, sh2, sc2, gt2 = ada_mod(c_ctx, ada_w[l], ada_b[l])
        y, kh, vh, s_f, s_b = mixers_context(modulate(xp, norm1_g[l], sh1, sc1), w_in[l], w_out[l], rw,
                                             pool_w[l], pool_scale[l])
        xp = xp + gt1 * y
        h = modulate(xp, norm2_g[l], sh2, sc2)
        if l % 2 == 0:
            f = swiglu(h, ffn_w1[i], ffn_w3[i], ffn_w2[i])
        else:
            f = moe_swiglu(h, moe_router[i], moe_w1[i], moe_w3[i], moe_w2[i])
        xp = xp + gt2 * f
        ks_new.append(kh)
        vs_new.append(vh)
        sf_new.append(s_f)
        sb_new.append(s_b)
        sh1, sc1, gt1, sh2, sc2, gt2 = ada_mod(c, ada_w[l], ada_b[l])
        y = mixers_latent(modulate(xs, norm1_g[l], sh1, sc1), cache_attn_k[:, l], cache_attn_v[:, l],
                          state_rwkv_fwd[:, l], state_rwkv_bwd[:, l], w_in[l], w_out[l], na_rpb[l], rw,
                          pool_w[l], pool_scale[l])
        xs = xs + gt1 * y
        h = modulate(xs, norm2_g[l], sh2, sc2)
        if l % 2 == 0:
            f = swiglu(h, ffn_w1[i], ffn_w3[i], ffn_w2[i])
        else:
            f = moe_swiglu(h, moe_router[i], moe_w1[i], moe_w3[i], moe_w2[i])
        xs = xs + gt2 * f
    y_prompt = rms_norm(xp, final_g)
    y_sample = rms_norm(xs, final_g)
    new_attn_k = jnp.stack(ks_new, axis=1)
    new_attn_v = jnp.stack(vs_new, axis=1)
    new_state_fwd = jnp.stack(sf_new, axis=1)
    new_state_bwd = jnp.stack(sb_new, axis=1)
    return (y_prompt, y_sample, new_attn_k, new_attn_v, new_state_fwd, new_state_bwd)
```

```python
import math
import numpy as np
import concourse.bass as bass
import concourse.mybir as mybir
from concourse.bass_utils import run_bass_kernel_spmd

F32 = mybir.dt.float32
BF16 = mybir.dt.bfloat16
AF = mybir.ActivationFunctionType
ALU = mybir.AluOpType
AX = mybir.AxisListType

D = 2048
KC = 16
P_IN = 5760
NPS = 4
SEQ = 256
TS = 4096
PAD = 16
SEGP = SEQ + 2 * PAD
SBASE = NPS * SEGP + PAD
NCOL = NPS * SEGP + TS + 2 * PAD
D_FF = 5632
D_FFE = 7168
NE = 8
WB = 512
DEPTH = 2


def pcol(s):
    return s * SEGP + PAD


class Slot:
    def __init__(self, sem):
        self.sem = sem
        self.cnt = 0


class Buf:
    def __init__(self, t):
        self.t = t
        self.w = []
        self.r = []
        self.slot = None

    @property
    def sem(self):
        return None if self.slot is None else self.slot.sem

    @property
    def cnt(self):
        return self.slot.cnt

    @cnt.setter
    def cnt(self, v):
        self.slot.cnt = v


class _Rec:
    def __init__(self):
        self.call = None

    def __getattr__(self, name):
        def f(*a, **k):
            self.call = (name, a, k)
            return None
        return f


def _freeze(fn):
    r = _Rec()
    fn(r)
    name, a, k = r.call
    return lambda e: getattr(e, name)(*a, **k)


class Prog:
    ENG = ["pe", "dve", "act", "pool", "sp"]

    def __init__(self, nc, es):
        self.nc = nc
        self.es = es
        self.h = {"pe": nc.tensor, "dve": nc.vector, "act": nc.scalar, "pool": nc.gpsimd, "sp": nc.sync}
        self.sem = {e: es.enter_context(nc.semaphore("es_" + e)) for e in self.ENG}
        self.tick = {e: 0 for e in self.ENG}
        self.waited = {e: {} for e in self.ENG}
        self.dsems = []
        self.nb = 0
        self.q = {e: [] for e in self.ENG}
        self.tes = es
        self.stack = []
        self.free_slots = []
        self.scope_bufs = [[]]

    def push(self):
        import contextlib
        self.stack.append(self.tes)
        self.tes = contextlib.ExitStack()
        self.tes.__enter__()
        self.scope_bufs.append([])

    def pop(self):
        self.tes.close()
        self.tes = self.stack.pop()
        for b in self.scope_bufs.pop():
            if b.slot is not None:
                self.free_slots.append(b.slot)
                b.slot = None

    def buf(self, shape, dt, name=None):
        self.nb += 1
        t = self.tes.enter_context(self.nc.sbuf_tensor("sb_" + (name or "b") + f"_{self.nb}", list(shape), dt))
        b = Buf(t)
        self.scope_bufs[-1].append(b)
        return b

    def psum(self, shape, dt=F32, name=None):
        self.nb += 1
        t = self.tes.enter_context(self.nc.psum_tensor((name or "p") + f"_{self.nb}", list(shape), dt))
        return Buf(t)

    def _wait(self, eng, sem, key, val):
        if self.waited[eng].get(key, 0) >= val:
            return
        self.waited[eng][key] = val
        self.q[eng].append(("w", sem, val))

    def _deps(self, eng, R, W):
        deps = []
        for b in R:
            deps += b.w
        for b in W:
            deps += b.w + b.r
        best = {}
        for d in deps:
            if d[0] == "e":
                if d[1] == eng and eng == "pe":
                    continue
                key = ("e", d[1])
                sem = self.sem[d[1]]
                skey = "e" + d[1]
            else:
                key = ("d", id(d[1].slot))
                sem = d[1].sem
                skey = "d%d" % id(d[1].slot)
            if key not in best or best[key][2] < d[2]:
                best[key] = (sem, skey, d[2])
        for (sem, skey, val) in best.values():
            self._wait(eng, sem, skey, val)

    def op(self, eng, fn, R=(), W=()):
        self._deps(eng, R, W)
        self.tick[eng] += 1
        self.q[eng].append(("o", _freeze(fn), self.sem[eng], 1))
        me = ("e", eng, self.tick[eng])
        for b in W:
            b.w = [me]
            b.r = []
        for b in R:
            if b not in W:
                b.r = [x for x in b.r if not (x[0] == "e" and x[1] == eng)] + [me]

    def dma(self, q, out, in_, R=(), W=(), **kw):
        sb = (list(W) + list(R))
        assert len(sb) >= 1
        owner = sb[0]
        if owner.slot is None:
            if self.free_slots:
                owner.slot = self.free_slots.pop()
            else:
                owner.slot = Slot(self.es.enter_context(self.nc.semaphore("ds%d" % len(self.dsems))))
                self.dsems.append(owner.slot)
        self._deps(q, R, W)
        owner.cnt += 16
        self.q[q].append(("o", (lambda e, out=out, in_=in_, kw=kw: e.dma_start(out=out, in_=in_, **kw)), owner.sem, 16))
        me = ("d", owner, owner.cnt)
        for b in W:
            b.w = [me]
            b.r = []
        for b in R:
            if b not in W:
                b.r = b.r + [me]
        return me

    def raw(self, eng, fn, sem, inc):
        self.q[eng].append(("o", _freeze(fn), sem, inc))

    def replay(self):
        nc = self.nc
        with nc.Block() as block:
            names = {"pe": "tensor", "dve": "vector", "act": "scalar", "pool": "gpsimd", "sp": "sync"}
            for eng in self.ENG:
                items = self.q[eng]

                def body(e, items=items):
                    for it in items:
                        if it[0] == "w":
                            e.wait_ge(it[1], it[2])
                        else:
                            ins = it[1](e)
                            ins.then_inc(it[2], it[3])
                getattr(block, names[eng])(body)

    def barrier(self):
        for e in self.ENG:
            for e2 in self.ENG:
                if e2 != e and self.tick[e2] > 0:
                    self._wait(e, self.sem[e2], "e" + e2, self.tick[e2])
            for o in self.dsems:
                if o.cnt > 0:
                    self._wait(e, o.sem, "d%d" % id(o), o.cnt)


def build(cfg):
    nc = bass.Bass("TRN2", target_bir_lowering=False)
    import contextlib
    es = contextlib.ExitStack()
    with es:
        _build(nc, es, cfg)
    return nc


def _build(nc, es, cfg):
    pg = Prog(nc, es)
    op, dma = pg.op, pg.dma

    def din(name, shape, dt=F32):
        return nc.dram_tensor(name, list(shape), dt, kind="ExternalInput")

    def dout(name, shape):
        return nc.dram_tensor(name, list(shape), F32, kind="ExternalOutput")

    def dscr(name, shape, dt=F32):
        return nc.dram_tensor(name, list(shape), dt)

    xp_in = din("x_prompt", [NPS * SEQ, D])
    xs_in = din("x_sample", [TS, D])
    ck_in = din("cache_k", [DEPTH, 256, 1024])
    cv_in = din("cache_v", [DEPTH, 256, 1024])
    sf_in = din("state_f", [DEPTH, 8, 64, 64])
    sb_in = din("state_b", [DEPTH, 8, 64, 64])
    cond_in = din("cond", [2, D])
    sel_in = din("sel", [128, 4])
    ident_in = din("ident", [128, 128])
    namask_in = din("namask", [128, 64])
    invcnt_in = din("invcnt", [2, 4, TS])
    esel_in = din("esel", [8, 8 * 128])
    vecs_in = din("vecs", [DEPTH, 84, 128])
    adab_in = din("ada_b", [DEPTH, 96, 128])
    fing_in = din("final_g", [16, 128])
    rpb_in = din("na_rpb", [DEPTH, 240, 31])
    rowv_in = din("rowv", [DEPTH, 8, 512])
    w2_in = din("rw_w2", [DEPTH, 2, 96, 512])
    a2_in = din("rw_a2", [DEPTH, 2, 96, 512])
    g2_in = din("rw_g2", [DEPTH, 256, 512])
    poolw_in = din("pool_w", [DEPTH, 4, 128, 128])
    router_in = din("router", [D, 8])

    big = {}

    def gathered(name, rows, cols):
        if name not in cfg["bigs"]:
            return None
        if cfg.get("nogather"):
            return din(name + "_fullin", [rows, cols])
        sh = din(name, [rows // 8, cols])
        xi = dscr(name + "_xi", [rows // 8, cols])
        full = dscr(name + "_full", [rows, cols])
        big[name] = (sh, xi, full)
        return full

    W_in = [gathered(f"w_in{l}", D, P_IN) for l in range(DEPTH)]
    W_out = [gathered(f"w_out{l}", D, D) for l in range(DEPTH)]
    W_ada = [gathered(f"ada_w{l}", D, 6 * D) for l in range(DEPTH)]
    W_f1 = gathered("ffn_w1", D, D_FF)
    W_f3 = gathered("ffn_w3", D, D_FF)
    W_f2 = gathered("ffn_w2", D_FF, D)
    W_m1 = [gathered(f"moe_w1_{i}", 2 * D, D_FFE) for i in range(4)]
    W_m3 = [gathered(f"moe_w3_{i}", 2 * D, D_FFE) for i in range(4)]
    W_m2 = [gathered(f"moe_w2_{i}", 2 * D_FFE, D) for i in range(4)]

    yp_out = dout("y_prompt", [NPS * SEQ, D])
    ys_out = dout("y_sample", [1024, D])
    nk_out = dout("new_k", [NPS * DEPTH * SEQ, 1024])
    nv_out = dout("new_v", [NPS * DEPTH * SEQ, 1024])
    nsf_out = dout("new_sf", [NPS, DEPTH, 8, 64, 64])
    nsb_out = dout("new_sb", [NPS, DEPTH, 8, 64, 64])

    XT = dscr("XT", [D, NCOL])
    QT = dscr("QT", [1024, NCOL], BF16)
    KT = dscr("KT", [1024, NCOL], BF16)
    VT = dscr("VT", [NCOL, 1024], BF16)
    RW = dscr("RW", [2176, NCOL])
    UP = dscr("UP", [512, NCOL])
    OT = dscr("OT", [D, NCOL], BF16)
    XO = dscr("XO", [D, 2048])
    TM = {k: dscr("TM_" + k, [NCOL, 512]) for k in ["dec0", "dec1", "b0", "b1", "kd0", "kd1", "kk", "r"]}
    VS = dscr("VS", [512, NCOL])
    BON = dscr("BON", [512, NCOL])
    GG = dscr("GG", [512, NCOL])
    OS = [dscr("OS0", [512, NCOL]), dscr("OS1", [512, NCOL])]
    RPAD = dscr("RPAD", [240, 64, 128])

    ident = pg.buf([128, 128], F32, "ident")
    identb = pg.buf([128, 128], BF16, "identb")
    ones = pg.buf([128, 128], F32, "ones")
    onesb = pg.buf([128, 64], BF16, "onesb")
    bdiag = pg.buf([128, 128], F32, "bdiag")
    zeros = pg.buf([128, 512], F32, "zeros")
    selt = pg.buf([128, 4], F32, "selt")
    dma("sp", ident.t[:, :], ident_in[:, :], W=[ident])
    dma("sp", selt.t[:, :], sel_in[:, :], W=[selt])
    op("dve", lambda e: e.tensor_copy(out=identb.t[:, :], in_=ident.t[:, :]), R=[ident], W=[identb])
    op("dve", lambda e: e.memset(ones.t[:, :], 1.0), W=[ones])
    op("dve", lambda e: e.memset(onesb.t[:, :], 1.0), W=[onesb])
    op("dve", lambda e: e.memset(bdiag.t[:, :], 0.0), W=[bdiag])
    op("dve", lambda e: e.memset(bdiag.t[0:64, 0:64], 1.0), W=[bdiag])
    op("dve", lambda e: e.memset(bdiag.t[64:128, 64:128], 1.0), W=[bdiag])
    op("dve", lambda e: e.memset(zeros.t[:, :], 0.0), W=[zeros])

    ccsem = es.enter_context(nc.semaphore("ccsem"))
    cpy = Slot(es.enter_context(nc.semaphore("cpysem")))
    pg.dsems.append(cpy)
    order = [f"ada_w0", "w_in0", "w_out0", "ffn_w1", "ffn_w3", "ffn_w2", "ada_w1", "w_in1", "w_out1",
             ] + [f"moe_w{k}_{i}" for i in range(4) for k in (1, 3, 2)]
    order = [n for n in order if n in cfg["bigs"] and not cfg.get("nogather")]
    ncc = 0
    for n in order:
        sh, xi, full = big[n]
        rows = sh.shape[0]
        step = max(1, rows // 4)
        for r0 in range(0, rows, step):
            r1 = min(rows, r0 + step)
            cpy.cnt += 16
            pg.raw("sp", (lambda e, o=xi[r0:r1, :], i=sh[r0:r1, :]: e.dma_start(out=o, in_=i)), cpy.sem, 16)
    pg._wait("pool", cpy.sem, "d%d" % id(cpy), cpy.cnt)
    for n in order:
        sh, xi, full = big[n]
        pg.raw("pool", (lambda e, xi=xi, full=full: e.collective_compute(
            "AllGather", ALU.bypass, replica_groups=[list(range(8))],
            ins=[xi.ap().opt()], outs=[full.ap().opt()])), ccsem, 1)
        ncc += 1
    for e in pg.ENG:
        pg.q[e].append(("w", ccsem, ncc))

    if cfg.get("stop_after") == "G":
        pg.barrier()
        pg.replay()
        return
    zsem_owner = zeros
    for (T_, rows) in [(RW, 1536), (UP, 512)]:
        for r0 in range(0, rows, 128):
            for c0 in range(0, NCOL, 512):
                c1 = min(NCOL, c0 + 512)
                dma("sp", T_[r0:r0 + 128, c0:c1], zeros.t[:, 0:c1 - c0], R=[zeros])
    for n in range(240):
        pass
    for n0 in range(0, 240, 4):
        dma("sp", RPAD[n0:n0 + 4, :, :].rearrange("n m c -> m n c"),
            zeros.t[0:64, :].rearrange("m (n c) -> m n c", c=128), R=[zeros])

    if cfg.get("stop_after") == "Z":
        pg.barrier()
        pg.replay()
        return
    pg.push()
    xin = [pg.buf([128, D], F32, f"xin{i}") for i in range(2)]
    xtp = [pg.psum([128, 512], F32, f"xtp{i}") for i in range(2)]
    xts = [pg.buf([128, 4, 128], F32, f"xts{i}") for i in range(2)]
    blocks = [(xp_in, s * SEQ + b * 128, pcol(s) + b * 128) for s in range(NPS) for b in range(2)]
    blocks += [(xs_in, b * 128, SBASE + b * 128) for b in range(TS // 128)]
    it = 0
    for bi, (src, r0, c0) in enumerate(blocks):
        xb = xin[bi % 2]
        dma("sp", xb.t[:, :], src[r0:r0 + 128, :], W=[xb])
        for g in range(4):
            ps = xtp[it % 2]
            st = xts[it % 2]
            it += 1
            for j in range(4):
                kc = g * 4 + j
                op("pe", lambda e, ps=ps, j=j, kc=kc, xb=xb: e.transpose(
                    out=ps.t[:, j * 128:(j + 1) * 128], in_=xb.t[:, kc * 128:(kc + 1) * 128], identity=ident.t[:, :]),
                   R=[xb, ident], W=[ps])
            op("act", lambda e, ps=ps, st=st: e.activation(
                out=st.t[:, :, :], in_=ps.t[:, :].rearrange("p (a b) -> p a b", b=128), func=AF.Copy),
               R=[ps], W=[st])
            dma("sp", XT[g * 512:(g + 1) * 512, c0:c0 + 128].rearrange("(a p) n -> p a n", p=128),
                st.t[:, :, :], R=[st])
    pg.barrier()
    pg.pop()


    if cfg.get("stop_after") == "X":
        pg.barrier()
        pg.replay()
        return

    def fm(T_, r0, nch, c0, N):
        return T_[r0:r0 + nch * 128, c0:c0 + N].rearrange("(a p) n -> p a n", p=128)

    NPS_ = 4
    ps4 = [pg.psum([128, 512], F32, f"ps{i}") for i in range(4)]
    pss = pg.psum([128, 512], F32, "pss")
    psctr = [0]

    def nps():
        psctr[0] += 1
        return ps4[psctr[0] % 4]

    xt = hT = sq = rstd = tmpn = None
    wt = [pg.buf([128, 16, 512], BF16, f"wt{i}") for i in range(2)]

    def work_alloc():
        nonlocal xt, hT, sq, rstd, tmpn
        pg.push()
        xt = pg.buf([128, 16, 512], F32, "xt")
        hT = pg.buf([128, 16, 1024], BF16, "hT")
        sq = [pg.buf([128, 512], F32, f"sq{i}") for i in range(2)]
        rstd = pg.buf([128, 512], F32, "rstd")
        tmpn = [pg.buf([128, 512], F32, f"tmpn{i}") for i in range(2)]

    def work_free():
        pg.barrier()
        pg.pop()
    stf = [pg.buf([128, 512], F32, f"stf{i}") for i in range(3)]
    stb = [pg.buf([128, 512], BF16, f"stb{i}") for i in range(3)]
    ctr = {"sq": 0, "tmpn": 0, "wt": 0, "stf": 0, "stb": 0}

    def rot(lst, key):
        ctr[key] += 1
        return lst[ctr[key] % len(lst)]

    VC = pg.buf([128, 84], F32, "VC")
    ADAB = pg.buf([128, 96], F32, "ADAB")
    FING = pg.buf([128, 16], F32, "FING")
    MOD = pg.buf([128, 96, 2], F32, "MOD")
    A1 = pg.buf([128, 16, 2], F32, "A1")
    A2 = pg.buf([128, 16, 2], F32, "A2")
    vload = pg.buf([128, 128], F32, "vload")
    CT = pg.buf([128, 2, 16], BF16, "CT")
    CTf = pg.buf([128, 2, 16], F32, "CTf")
    epsc = pg.buf([128, 1], F32, "epsc")
    op("dve", lambda e: e.memset(epsc.t[:, :], 1e-6), W=[epsc])

    def vec_cols(src_ap, nrows, dst):
        dma("sp", vload.t[0:nrows, :], src_ap, W=[vload])
        op("pe", lambda e: e.transpose(out=pss.t[:, 0:nrows], in_=vload.t[0:nrows, :], identity=ident.t[0:nrows, 0:nrows]),
           R=[vload, ident], W=[pss])
        op("dve", lambda e: e.tensor_copy(out=dst.t[:, 0:nrows], in_=pss.t[:, 0:nrows]), R=[pss], W=[dst])

    vec_cols(fing_in[:, :], 16, FING)
    dma("sp", CTf.t[:, :, :], cond_in.ap().rearrange("j (p k) -> p j k", k=16), W=[CTf])
    op("act", lambda e: e.activation(out=CT.t[:, :, :], in_=CTf.t[:, :, :], func=AF.Silu), R=[CTf], W=[CT])

    def compute_mods(l):
        vec_cols(vecs_in[l, :, :], 84, VC)
        vec_cols(adab_in[l, :, :], 96, ADAB)
        wa = W_ada[l]
        for ti in range(24):
            w = rot(wt, "wt")
            dma("pool", w.t[:, :, :], wa[:, ti * 512:(ti + 1) * 512].rearrange("(p k) n -> p k n", k=16), W=[w])
            for nc_ in range(4):
                j = ti * 4 + nc_
                for kc in range(16):
                    op("pe", lambda e, w=w, nc_=nc_, kc=kc, j=j: e.matmul(
                        pss.t[:, 2 * j:2 * j + 2], lhsT=w.t[:, kc, nc_ * 128:(nc_ + 1) * 128], rhs=CT.t[:, :, kc],
                        start=(kc == 0), stop=(kc == 15)), R=[w, CT], W=[pss])
        for j in range(2):
            op("dve", lambda e, j=j: e.tensor_tensor(
                out=MOD.t[:, :, j], in0=pss.t[:, 0:192].rearrange("p (a b) -> p a b", b=2)[:, :, j], in1=ADAB.t[:, :], op=ALU.add),
               R=[pss, ADAB], W=[MOD])
        for (A, g0, s0) in [(A1, 0, 16), (A2, 16, 64)]:
            for j in range(2):
                op("dve", lambda e, A=A, g0=g0, s0=s0, j=j: e.scalar_tensor_tensor(
                    out=A.t[:, :, j], in0=MOD.t[:, s0:s0 + 16, j], scalar=1.0, in1=VC.t[:, g0:g0 + 16],
                    op0=ALU.add, op1=ALU.mult), R=[MOD, VC], W=[A])

    def normmod(xsrc_ap, N, Acol, Bcol, out_fn):
        for kc in range(16):
            s_ = rot(sq, "sq")
            op("act", lambda e, s_=s_, kc=kc: e.activation(out=s_.t[:, 0:N], in_=xsrc_ap(kc), func=AF.Square), R=[xt], W=[s_])
            op("pe", lambda e, s_=s_, kc=kc: e.matmul(pss.t[:, 0:N], lhsT=ones.t[:, :], rhs=s_.t[:, 0:N],
                                                   start=(kc == 0), stop=(kc == 15)), R=[s_, ones], W=[pss])
        op("act", lambda e: e.activation(out=rstd.t[:, 0:N], in_=pss.t[:, 0:N], func=AF.Sqrt, scale=1.0 / D, bias=epsc.t[:, 0:1]),
           R=[pss, epsc], W=[rstd])
        op("dve", lambda e: e.reciprocal(out=rstd.t[:, 0:N], in_=rstd.t[:, 0:N]), R=[rstd], W=[rstd])
        for kc in range(16):
            t_ = rot(tmpn, "tmpn")
            op("dve", lambda e, t_=t_, kc=kc: e.tensor_tensor(out=t_.t[:, 0:N], in0=xsrc_ap(kc), in1=rstd.t[:, 0:N], op=ALU.mult),
               R=[xt, rstd], W=[t_])
            out_ap, out_bufs = out_fn(kc)
            if Bcol is None:
                op("act", lambda e, t_=t_, kc=kc, out_ap=out_ap: e.activation(out=out_ap, in_=t_.t[:, 0:N], func=AF.Copy, scale=Acol(kc)),
                   R=[t_], W=out_bufs)
            else:
                op("dve", lambda e, t_=t_, kc=kc, out_ap=out_ap: e.tensor_scalar(
                    out=out_ap, in0=t_.t[:, 0:N], scalar1=Acol(kc), scalar2=Bcol(kc), op0=ALU.mult, op1=ALU.add),
                   R=[t_], W=out_bufs)

    SUS = [([(pcol(s), SEQ) for s in range(NPS)], 0)]
    for q in range(4):
        SUS.append(([(SBASE + q * 1024 + i * 512, 512) for i in range(2)], 1))

    def load_x(c0, N):
        dma("sp", xt.t[:, :, 0:N], fm(XT, 0, 16, c0, N), W=[xt])

    def linear_fm(rhsbuf, pieces, Wd, row0, kcn, coltiles, epi, skip=None):
        for (c0, ncols) in coltiles:
            w = rot(wt, "wt")
            dma("pool", w.t[:, 0:kcn, 0:ncols], Wd[row0:row0 + kcn * 128, c0:c0 + ncols].rearrange("(a p) n -> p a n", p=128), W=[w])
            for pi, (off, N) in enumerate(pieces):
                for nc_ in range(ncols // 128):
                    ps = nps()
                    for kc in range(kcn):
                        op("pe", lambda e, ps=ps, w=w, kc=kc, nc_=nc_, off=off, N=N: e.matmul(
                            ps.t[:, 0:N], lhsT=w.t[:, kc, nc_ * 128:(nc_ + 1) * 128], rhs=rhsbuf.t[:, kc, off:off + N],
                            start=(kc == 0), stop=(kc == kcn - 1)), R=[w, rhsbuf], W=[ps])
                    epi(ps, (c0 // 128) + nc_, pi, N)

    def phaseA(l):
        work_alloc()
        for (pieces, j) in SUS[:cfg.get("A_sus", 5)]:
            offs = []
            off = 0
            for (c0, N) in pieces:
                load_x(c0, N)
                normmod(lambda kc, N=N: xt.t[:, kc, 0:N], N,
                        lambda kc: A1.t[:, kc, j:j + 1], lambda kc: MOD.t[:, kc, j:j + 1],
                        lambda kc, off=off, N=N: (hT.t[:, kc, off:off + N], [hT]))
                offs.append((off, N))
                off += N

            def epi(ps, gc, pi, N, pieces=pieces):
                c0 = pieces[pi][0]
                if gc < 16:
                    s_ = rot(stb, "stb")
                    op("act", lambda e: e.activation(out=s_.t[:, 0:N], in_=ps.t[:, 0:N], func=AF.Copy), R=[ps], W=[s_])
                    T_ = QT if gc < 8 else KT
                    r0 = (gc % 8) * 128
                    dma("sp", T_[r0:r0 + 128, c0:c0 + N], s_.t[:, 0:N], R=[s_])
                else:
                    s_ = rot(stf, "stf")
                    op("act", lambda e: e.activation(out=s_.t[:, 0:N], in_=ps.t[:, 0:N], func=AF.Copy), R=[ps], W=[s_])
                    if gc < 41:
                        r0 = (gc - 24) * 128
                        dma("sp", RW[r0:r0 + 128, c0:c0 + N], s_.t[:, 0:N], R=[s_])
                    else:
                        r0 = (gc - 41) * 128
                        dma("sp", UP[r0:r0 + 128, c0:c0 + N], s_.t[:, 0:N], R=[s_])

            tiles = [(c, 512) for c in range(0, 2048, 512)] + [(c, 512) for c in range(3072, 5632, 512)] + [(5632, 128)]
            if not cfg.get("A_skip_linear"):
                linear_fm(hT, offs, W_in[l], 0, 16, tiles, epi)
            kinds = [(2048, "v")] + ([(1024, "k")] if j == 0 else [])
            if cfg.get("A_skip_tm"):
                kinds = []
            for (cbase, kind) in kinds[:cfg.get("tm_kinds", 2)]:
                for half in range(cfg.get("tm_halves", 2)):
                    w = rot(wt, "wt")
                    dma("pool", w.t[:, :, :], W_in[l][:, cbase + half * 512:cbase + (half + 1) * 512].rearrange("(a p) n -> p a n", p=128), W=[w])
                    for pi, (off, N) in enumerate(offs[:cfg.get("tm_pieces", 8)]):
                        c0 = pieces[pi][0]
                        for b in range(N // 128):
                            ps = nps()
                            for kc in range(16):
                                op("pe", lambda e, ps=ps, w=w, kc=kc, o=off + b * 128: e.matmul(
                                    ps.t[:, :], lhsT=hT.t[:, kc, o:o + 128], rhs=w.t[:, kc, :],
                                    start=(kc == 0), stop=(kc == 15)), R=[w, hT], W=[ps])
                            if kind == "v":
                                s_ = rot(stb, "stb")
                                op("act", lambda e, s_=s_, ps=ps: e.activation(out=s_.t[:, :], in_=ps.t[:, :], func=AF.Copy), R=[ps], W=[s_])
                                dma("sp", VT[c0 + b * 128:c0 + (b + 1) * 128, half * 512:(half + 1) * 512], s_.t[:, :], R=[s_])
                            if j == 0 and not cfg.get("A_skip_out"):
                                s2 = rot(stf, "stf")
                                if True:
                                    op("act", lambda e, s2=s2, ps=ps: e.activation(out=s2.t[:, :], in_=ps.t[:, :], func=AF.Copy), R=[ps], W=[s2])
                                else:
                                    op("dve", lambda e, s2=s2, ps=ps: e.tensor_copy(out=s2.t[:, :], in_=ps.t[:, :]), R=[ps], W=[s2])
                                dst = nv_out if kind == "v" else nk_out
                                orow = (pi * DEPTH + l) * SEQ + b * 128
                                dma("sp", dst[orow:orow + 128, half * 512:(half + 1) * 512], s2.t[:, :], R=[s2])
        work_free()


    OFFP = 16
    actr = {"s": 0, "p": 0, "o": 0}

    def attention(l):
        pg.push()
        OFFP = 16
        cvb = pg.buf([128, 2, 1024], BF16, "cvb")
        ckT = pg.buf([128, 8, 256], BF16, "ckT")
        pg.push()
        ckf = pg.buf([128, 2, 1024], F32, "ckf")
        rpf = pg.buf([120, 2, 31], F32, "rpf")
        rpr = pg.buf([120, 2, 31], F32, "rpr")
        dma("sp", rpf.t[:, :, :], rpb_in[l, :, :].rearrange("(a n) d -> n a d", a=2), W=[rpf])
        for d_ in range(31):
            op("pool", lambda e, d_=d_: e.tensor_copy(out=rpr.t[:, :, d_:d_ + 1], in_=rpf.t[:, :, 30 - d_:31 - d_]), R=[rpf], W=[rpr])
        for a in range(2):
            dst = bass.AP(tensor=RPAD, offset=a * 120 * 8192 + (OFFP - 15), ap=[[8192, 120], [129, 64], [1, 31]])
            dma("sp", dst, rpr.t[:, a, :].unsqueeze(1).broadcast_to([120, 64, 31]), R=[rpr])
        dma("sp", ckf.t[:, :, :], ck_in[l, :, :].rearrange("(b p) d -> p b d", p=128), W=[ckf])
        for hp in range(8):
            for b in range(2):
                op("pe", lambda e, hp=hp, b=b: e.transpose(out=pss.t[:, b * 128:(b + 1) * 128], in_=ckf.t[:, b, hp * 128:(hp + 1) * 128],
                                                          identity=ident.t[:, :]), R=[ckf, ident], W=[pss])
            op("act", lambda e, hp=hp: e.activation(out=ckT.t[:, hp, :], in_=pss.t[:, 0:256], func=AF.Copy), R=[pss], W=[ckT])
        dma("pool", cvb.t[:, :, :], cv_in[l, :, :].rearrange("(b p) d -> p b d", p=128), W=[cvb])
        pg.barrier()
        pg.pop()
        qT = pg.buf([128, NCOL], BF16, "qT")
        kT = pg.buf([128, NCOL], BF16, "kT")
        vpr = pg.buf([128, 8, 128], BF16, "vpr")
        vse = pg.buf([128, 32, 128], BF16, "vse")
        vso = pg.buf([128, 32, 128], BF16, "vso")
        biasb = pg.buf([128, 2, 9, 384], F32, "biasb")
        maskt = pg.buf([128, 64], F32, "maskt")
        sTt = [pg.buf([128, 384], F32, f"sTt{i}") for i in range(2)]
        pTt = [pg.buf([128, 512], BF16, f"pTt{i}") for i in range(2)]
        rDt = pg.buf([128, 256], F32, "rDt")
        oacc = [pg.buf([128, 512], BF16, f"oacc{i}") for i in range(2)]
        dma("sp", maskt.t[:, :], namask_in[:, :], W=[maskt])
        for hp in range(cfg.get("att_hps", 8)):
            for (sc0, sn) in [(pcol(i_), SEQ) for i_ in range(NPS)] + [(SBASE, TS)]:
                dma("sp", qT.t[:, sc0:sc0 + sn], QT[hp * 128:(hp + 1) * 128, sc0:sc0 + sn], W=[qT])
                dma("sp", kT.t[:, sc0:sc0 + sn], KT[hp * 128:(hp + 1) * 128, sc0:sc0 + sn], W=[kT])
            for s_i in range(NPS):
                dma("sp", vpr.t[:, 2 * s_i:2 * s_i + 2, :],
                    VT[pcol(s_i):pcol(s_i) + 256, hp * 128:(hp + 1) * 128].rearrange("(b p) d -> p b d", p=128), W=[vpr])
            dma("sp", vse.t[:, :, :], VT[SBASE:SBASE + 4096, hp * 128:(hp + 1) * 128].rearrange("(b p) d -> p b d", p=128), W=[vse])
            dma("sp", vso.t[:, 0:31, :], VT[SBASE + 64:SBASE + 64 + 31 * 128, hp * 128:(hp + 1) * 128].rearrange("(b p) d -> p b d", p=128), W=[vso])
            op("pool", lambda e: e.memset(biasb.t[:, :, :, :], 0.0), W=[biasb])
            for hh in range(2):
                h = hp * 2 + hh
                for pat in range(9):
                    for ck_ in range(4):
                        kr = 2 * ck_
                        if pat < 4:
                            dri = kr - pat + 7
                        elif pat == 4:
                            dri = kr + 3
                        else:
                            dri = kr + 63 - (60 + pat - 5)
                        n = h * 15 + dri
                        dma("sp", biasb.t[:, hh, pat, ck_ * 64:(ck_ + 1) * 64],
                            RPAD[n:n + 2, :, OFFP:OFFP + 64].rearrange("n m c -> (n m) c"), W=[biasb])
            for hh in range(2):
                for pat in range(9):
                    op("pool", lambda e, hh=hh, pat=pat: e.tensor_tensor(
                        out=biasb.t[:, hh, pat, 0:256].rearrange("p (a c) -> p a c", c=64),
                        in0=biasb.t[:, hh, pat, 0:256].rearrange("p (a c) -> p a c", c=64),
                        in1=maskt.t[:, :].unsqueeze(1).broadcast_to([128, 4, 64]), op=ALU.add), R=[maskt], W=[biasb])

            def unit(qc0, N, chunks, bias_fn, out_ap_fn, out_bufs):
                nchk = len(chunks)
                psO = nps()
                psD = nps()
                for hh in range(2):
                    pb = 64 * hh
                    pS = nps()
                    for ci, (kf, vf) in enumerate(chunks):
                        op("pe", lambda e, pS=pS, ci=ci, kf=kf, hh=hh, pb=pb: e.matmul(
                            pS.t[:, ci * N:(ci + 1) * N], lhsT=kf(hh), rhs=qT.t[pb:pb + 64, qc0:qc0 + N], start=True, stop=True),
                           R=[kT, qT, ckT], W=[pS])
                    actr["p"] += 1
                    pT = pTt[actr["p"] % 2]
                    if bias_fn is not None:
                        actr["s"] += 1
                        sT = sTt[actr["s"] % 2]
                        op("dve", lambda e, sT=sT, pS=pS, hh=hh: e.scalar_tensor_tensor(
                            out=sT.t[:, 0:nchk * N], in0=pS.t[:, 0:nchk * N], scalar=0.125, in1=bias_fn(hh),
                            op0=ALU.mult, op1=ALU.add), R=[pS, biasb], W=[sT])
                        op("act", lambda e, sT=sT, pT=pT: e.activation(out=pT.t[:, 0:nchk * N], in_=sT.t[:, 0:nchk * N], func=AF.Exp),
                           R=[sT], W=[pT])
                    else:
                        op("act", lambda e, pS=pS, pT=pT: e.activation(out=pT.t[:, 0:nchk * N], in_=pS.t[:, 0:nchk * N], func=AF.Exp, scale=0.125),
                           R=[pS], W=[pT])
                    for ci, (kf, vf) in enumerate(chunks):
                        op("pe", lambda e, ci=ci, vf=vf, hh=hh, pb=pb, pT=pT: e.matmul(
                            psO.t[pb:pb + 64, 0:N], lhsT=vf(hh), rhs=pT.t[:, ci * N:(ci + 1) * N], start=(ci == 0), stop=(ci == nchk - 1)),
                           R=[pT, vpr, vse, vso, cvb], W=[psO])
                        op("pe", lambda e, ci=ci, hh=hh, pb=pb, pT=pT: e.matmul(
                            psD.t[pb:pb + 64, 0:N], lhsT=onesb.t[:, :], rhs=pT.t[:, ci * N:(ci + 1) * N], start=(ci == 0), stop=(ci == nchk - 1)),
                           R=[pT, onesb], W=[psD])
                op("dve", lambda e: e.reciprocal(out=rDt.t[:, 0:N], in_=psD.t[:, 0:N]), R=[psD], W=[rDt])
                op("dve", lambda e: e.tensor_tensor(out=out_ap_fn(), in0=psO.t[:, 0:N], in1=rDt.t[:, 0:N], op=ALU.mult),
                   R=[psO, rDt], W=out_bufs)

            for s_i in range(NPS if not cfg.get("att_skip_prompt") else 0):
                c0 = pcol(s_i)
                actr["o"] += 1
                oa_ = oacc[actr["o"] % 2]
                chunks = []
                for kb in range(2):
                    chunks.append((lambda hh, kb=kb, c0=c0: kT.t[64 * hh:64 * hh + 64, c0 + kb * 128:c0 + (kb + 1) * 128],
                                   lambda hh, kb=kb, s_i=s_i: vpr.t[:, 2 * s_i + kb, 64 * hh:64 * hh + 64]))
                unit(c0, 256, chunks, None, lambda oa_=oa_: oa_.t[:, 0:256], [oa_])
                dma("sp", OT[hp * 128:(hp + 1) * 128, c0:c0 + 256], oa_.t[:, 0:256], R=[oa_])
            for r in cfg.get("att_rowlist", range(64)):
                rs = min(max(r - 4, 0), 56)
                pat = r if r < 4 else (4 if r < 60 else 5 + r - 60)
                if r % 8 == 0:
                    actr["o"] += 1
                    oa_ = oacc[actr["o"] % 2]
                chunks = []
                for ck_ in range(4):
                    row0 = rs + 2 * ck_
                    kc0 = SBASE + 64 * row0
                    vt_, blk = (vse, row0 // 2) if row0 % 2 == 0 else (vso, (row0 - 1) // 2)
                    chunks.append((lambda hh, kc0=kc0: kT.t[64 * hh:64 * hh + 64, kc0:kc0 + 128],
                                   lambda hh, vt_=vt_, blk=blk: vt_.t[:, blk, 64 * hh:64 * hh + 64]))
                for kb in range(2):
                    chunks.append((lambda hh, kb=kb: ckT.t[64 * hh:64 * hh + 64, hp, kb * 128:(kb + 1) * 128],
                                   lambda hh, kb=kb: cvb.t[:, kb, (hp * 2 + hh) * 64:(hp * 2 + hh + 1) * 64]))
                oc_ = (r % 8) * 64
                unit(SBASE + 64 * r, 64, chunks, lambda hh, pat=pat: biasb.t[:, hh, pat, :],
                     lambda oa_=oa_, oc_=oc_: oa_.t[:, oc_:oc_ + 64], [oa_])
                if r % 8 == 7:
                    cc0 = SBASE + 64 * (r - 7)
                    dma("sp", OT[hp * 128:(hp + 1) * 128, cc0:cc0 + 512], oa_.t[:, :], R=[oa_])
        pg.barrier()
        pg.pop()


    def pool_phase(l):
        pg.push()
        ub = pg.buf([128, 544], F32, "ub")
        la = pg.buf([128, 544], F32, "la")
        lb = pg.buf([128, 544], F32, "lb")
        icb = pg.buf([128, 512], F32, "icb")
        pl = pg.buf([128, 512], F32, "pl")
        plb = pg.buf([128, 512], BF16, "plb")
        pw = pg.buf([128, 4, 128], BF16, "pw")
        dma("pool", pw.t[:, :, :], poolw_in[l, :, :, :].rearrange("g c d -> c g d"), W=[pw])
        segs = [(pcol(s_), SEQ, 0, 0) for s_ in range(NPS)] + [(SBASE + i * 512, 512, 1, i * 512) for i in range(8)]
        for (c0, N, si, t0) in segs:
            L = N + 32
            for g in range(4):
                dma("sp", ub.t[:, 0:L], UP[g * 128:(g + 1) * 128, c0 - 16:c0 + N + 16], W=[ub])
                dma("sp", icb.t[:, 0:N], invcnt_in[si, g, t0:t0 + N].partition_broadcast(128), W=[icb])
                src = ub
                bufs = [la, lb]
                sh = 1
                for k in range(g + 1):
                    dst = bufs[k % 2]
                    if k == 0:
                        op("dve", lambda e, dst=dst: e.tensor_tensor(out=dst.t[:, 1:L], in0=ub.t[:, 0:L - 1], in1=ub.t[:, 1:L], op=ALU.add),
                           R=[ub], W=[dst])
                        lo, hi = 1, L
                    else:
                        d_ = 1 << (k - 1)
                        nlo, nhi = lo + d_, hi - d_
                        op("dve", lambda e, dst=dst, src=src, d_=d_, nlo=nlo, nhi=nhi: e.tensor_tensor(
                            out=dst.t[:, nlo:nhi], in0=src.t[:, nlo - d_:nhi - d_], in1=src.t[:, nlo + d_:nhi + d_], op=ALU.add),
                           R=[src], W=[dst])
                        lo, hi = nlo, nhi
                    src = dst
                op("dve", lambda e, src=src: e.tensor_tensor(out=pl.t[:, 0:N], in0=src.t[:, 16:16 + N], in1=icb.t[:, 0:N], op=ALU.mult),
                   R=[src, icb], W=[pl])
                op("dve", lambda e: e.tensor_tensor(out=plb.t[:, 0:N], in0=pl.t[:, 0:N], in1=ub.t[:, 16:16 + N], op=ALU.subtract),
                   R=[pl, ub], W=[plb])
                ps = nps()
                op("pe", lambda e, ps=ps, g=g: e.matmul(ps.t[:, 0:N], lhsT=pw.t[:, g, :], rhs=plb.t[:, 0:N], start=True, stop=True),
                   R=[pw, plb], W=[ps])
                s_ = rot(stb, "stb")
                op("act", lambda e, s_=s_, ps=ps, g=g: e.activation(out=s_.t[:, 0:N], in_=ps.t[:, 0:N], func=AF.Copy, scale=VC.t[:, 80 + g:81 + g]),
                   R=[ps, VC], W=[s_])
                dma("sp", OT[1536 + g * 128:1536 + (g + 1) * 128, c0:c0 + N], s_.t[:, 0:N], R=[s_])
        pg.barrier()
        pg.pop()

    xres = [pg.buf([128, 512], F32, f"xres{i}") for i in range(2)]
    yres = [pg.buf([128, 512], F32, f"yres{i}") for i in range(2)]
    ctr["xres"] = 0
    ctr["yres"] = 0

    def resid_epi(gate0, j, pieces_cols, XT=XT):
        def epi(ps, gc, pi, N):
            c0 = pieces_cols[pi]
            xr = rot(xres, "xres")
            yr = rot(yres, "yres")
            dma("sp", xr.t[:, 0:N], XT[gc * 128:(gc + 1) * 128, c0:c0 + N], W=[xr])
            op("act", lambda e: e.activation(out=yr.t[:, 0:N], in_=ps.t[:, 0:N], func=AF.Copy, scale=MOD.t[:, gate0 + gc, j:j + 1]),
               R=[ps, MOD], W=[yr])
            op("pool", lambda e: e.tensor_tensor(out=xr.t[:, 0:N], in0=xr.t[:, 0:N], in1=yr.t[:, 0:N], op=ALU.add), R=[xr, yr], W=[xr])
            dma("sp", XT[gc * 128:(gc + 1) * 128, c0:c0 + N], xr.t[:, 0:N], R=[xr])
        return epi

    def wout_phase(l):
        work_alloc()
        for (pieces, j) in SUS:
            offs = []
            off = 0
            for (c0, N) in pieces:
                dma("sp", hT.t[:, :, off:off + N], fm(OT, 0, 16, c0, N), W=[hT])
                offs.append((off, N))
                off += N
            linear_fm(hT, offs, W_out[l], 0, 16, [(c, 512) for c in range(0, D, 512)], resid_epi(32, j, [p_[0] for p_ in pieces]))
        work_free()

    def ffn_phase(l):
        work_alloc()
        pg.push()
        actT = pg.buf([128, 44, 512], BF16, "actT")
        t1 = [pg.buf([128, 512], F32, f"t1_{i}") for i in range(2)]
        t3 = [pg.buf([128, 512], F32, f"t3_{i}") for i in range(2)]
        ctr["t1"] = 0
        ctr["t3"] = 0
        for (pieces, j) in SUS:
            for (c0, N) in pieces:
                load_x(c0, N)
                normmod(lambda kc, N=N: xt.t[:, kc, 0:N], N,
                        lambda kc: A2.t[:, kc, j:j + 1], lambda kc: MOD.t[:, 48 + kc, j:j + 1],
                        lambda kc, N=N: (hT.t[:, kc, 0:N], [hT]))
                for ti in range(D_FF // 512):
                    w1 = rot(wt, "wt")
                    dma("pool", w1.t[:, :, :], W_f1[:, ti * 512:(ti + 1) * 512].rearrange("(a p) n -> p a n", p=128), W=[w1])
                    w3 = rot(wt, "wt")
                    dma("pool", w3.t[:, :, :], W_f3[:, ti * 512:(ti + 1) * 512].rearrange("(a p) n -> p a n", p=128), W=[w3])
                    for nc_ in range(4):
                        p1 = nps()
                        p3 = nps()
                        for (pp, ww) in [(p1, w1), (p3, w3)]:
                            for kc in range(16):
                                op("pe", lambda e, pp=pp, ww=ww, kc=kc, nc_=nc_: e.matmul(
                                    pp.t[:, 0:N], lhsT=ww.t[:, kc, nc_ * 128:(nc_ + 1) * 128], rhs=hT.t[:, kc, 0:N],
                                    start=(kc == 0), stop=(kc == 15)), R=[ww, hT], W=[pp])
                        a1 = rot(t1, "t1")
                        a3 = rot(t3, "t3")
                        op("act", lambda e, a1=a1, p1=p1: e.activation(out=a1.t[:, 0:N], in_=p1.t[:, 0:N], func=AF.Silu), R=[p1], W=[a1])
                        op("act", lambda e, a3=a3, p3=p3: e.activation(out=a3.t[:, 0:N], in_=p3.t[:, 0:N], func=AF.Copy), R=[p3], W=[a3])
                        fc = ti * 4 + nc_
                        op("dve", lambda e, a1=a1, a3=a3, fc=fc: e.tensor_tensor(out=actT.t[:, fc, 0:N], in0=a1.t[:, 0:N], in1=a3.t[:, 0:N], op=ALU.mult),
                           R=[a1, a3], W=[actT])
                epi = resid_epi(80, j, [c0])
                for n_ in range(16):
                    w2 = rot(wt, "wt")
                    w2v = w2.t[:, :, :].rearrange("p a b -> p (a b)")[:, 0:44 * 128].rearrange("p (k n) -> p k n", n=128)
                    dma("pool", w2v, W_f2[:, n_ * 128:(n_ + 1) * 128].rearrange("(k p) n -> p k n", p=128), W=[w2])
                    ps = nps()
                    for kc in range(44):
                        op("pe", lambda e, ps=ps, w2v=w2v, kc=kc: e.matmul(ps.t[:, 0:N], lhsT=w2v[:, kc, :], rhs=actT.t[:, kc, 0:N],
                                                                         start=(kc == 0), stop=(kc == 43)), R=[w2, actT], W=[ps])
                    epi(ps, n_, 0, N)
        pg.barrier()
        pg.pop()
        work_free()


    SEGS = [(pcol(s_), SEQ) for s_ in range(NPS)] + [(SBASE + i * 512, 512) for i in range(8)]
    GN_EPS = 64e-5

    def rwkv_prep(l):
        pg.push()
        rkvx = pg.buf([128, 12, 514], F32, "rkvx")
        rkvc = pg.buf([128, 12, 512], F32, "rkvc")
        tA = pg.buf([128, 512], F32, "tA")
        tB = pg.buf([128, 512], F32, "tB")
        tC = pg.buf([128, 512], F32, "tC")
        tD = pg.buf([128, 512], F32, "tD")
        rtm = pg.buf([128, 512], F32, "rtm")
        ktm = pg.buf([128, 512], F32, "ktm")
        kkt = pg.buf([128, 512], F32, "kkt")
        ss8 = pg.buf([128, 8], F32, "ss8")
        xg = pg.buf([128, 2, 512], F32, "xg")
        th = [pg.buf([97, 512], F32, f"th{i}") for i in range(4)]
        w2a = [pg.buf([97, 512], F32, f"w2a{i}") for i in range(4)]
        g2t = pg.buf([128, 2, 512], F32, "g2t")
        KKR = pg.buf([128, 512], F32, "KKR")
        KAR = pg.buf([128, 512], F32, "KAR")
        OMK = pg.buf([128, 512], F32, "OMK")
        eps12 = pg.buf([128, 1], F32, "eps12")
        op("dve", lambda e: e.memset(eps12.t[:, :], 1e-12), W=[eps12])
        for d_ in range(2):
            dma("sp", w2a[d_].t[0:96, :], w2_in[l, d_, :, :], W=[w2a[d_]])
            dma("sp", w2a[d_].t[96:97, :], rowv_in[l, d_:d_ + 1, :], W=[w2a[d_]])
            dma("sp", w2a[2 + d_].t[0:96, :], a2_in[l, d_, :, :], W=[w2a[2 + d_]])
            dma("sp", w2a[2 + d_].t[96:97, :], rowv_in[l, 2 + d_:3 + d_, :], W=[w2a[2 + d_]])
        dma("sp", g2t.t[:, :, :], g2_in[l, :, :].rearrange("(a p) n -> p a n", p=128), W=[g2t])
        dma("sp", KKR.t[:, :], rowv_in[l, 4, :].partition_broadcast(128), W=[KKR])
        dma("sp", KAR.t[:, :], rowv_in[l, 5, :].partition_broadcast(128), W=[KAR])
        op("dve", lambda e: e.tensor_scalar(out=OMK.t[:, :], in0=KAR.t[:, :], scalar1=-1.0, scalar2=1.0, op0=ALU.mult, op1=ALU.add),
           R=[KAR], W=[OMK])
        for i_ in range(4):
            op("dve", lambda e, i_=i_: e.memset(th[i_].t[96:97, :], 1.0), W=[th[i_]])
        CV = lambda tap, c: VC.t[:, 32 + tap * 12 + c:33 + tap * 12 + c]
        for (c0, N) in SEGS[:cfg.get("rw_segs", 12)]:
            dma("sp", rkvx.t[:, :, 0:N + 2], fm(RW, 0, 12, c0 - 1, N + 2), W=[rkvx])
            for c in range(12):
                op("act", lambda e, c=c: e.activation(out=rkvc.t[:, c, 0:N], in_=rkvx.t[:, c, 0:N], func=AF.Copy, scale=CV(0, c)),
                   R=[rkvx, VC], W=[rkvc])
                for tap in (1, 2):
                    op("dve", lambda e, c=c, tap=tap: e.scalar_tensor_tensor(
                        out=rkvc.t[:, c, 0:N], in0=rkvx.t[:, c, tap:tap + N], scalar=CV(tap, c), in1=rkvc.t[:, c, 0:N],
                        op0=ALU.mult, op1=ALU.add), R=[rkvx, VC, rkvc], W=[rkvc])
            dma("sp", fm(VS, 0, 4, c0, N), rkvc.t[:, 8:12, 0:N], R=[rkvc])
            for c in range(4):
                op("dve", lambda e, c=c: e.scalar_tensor_tensor(out=tA.t[:, 0:N], in0=rkvc.t[:, c, 0:N], scalar=VC.t[:, 68 + c:69 + c],
                                                               in1=rkvc.t[:, 4 + c, 0:N], op0=ALU.mult, op1=ALU.mult), R=[rkvc, VC], W=[tA])
                ps = nps()
                op("pe", lambda e, ps=ps: e.matmul(ps.t[:, 0:N], lhsT=bdiag.t[:, :], rhs=tA.t[:, 0:N], start=True, stop=True), R=[bdiag, tA], W=[ps])
                op("act", lambda e, ps=ps: e.activation(out=tB.t[:, 0:N], in_=ps.t[:, 0:N], func=AF.Copy), R=[ps], W=[tB])
                op("dve", lambda e, c=c: e.tensor_tensor(out=tB.t[:, 0:N], in0=tB.t[:, 0:N], in1=rkvc.t[:, 8 + c, 0:N], op=ALU.mult), R=[tB, rkvc], W=[tB])
                dma("sp", BON[c * 128:(c + 1) * 128, c0:c0 + N], tB.t[:, 0:N], R=[tB])
            dma("sp", xg.t[:, :, 0:N], fm(RW, 1920, 2, c0, N), W=[xg])
            op("act", lambda e: e.activation(out=xg.t[:, :, 0:N], in_=xg.t[:, :, 0:N], func=AF.Sigmoid), R=[xg], W=[xg])
            for c in range(4):
                ps = nps()
                for kc in range(2):
                    op("pe", lambda e, ps=ps, c=c, kc=kc: e.matmul(ps.t[:, 0:N], lhsT=g2t.t[:, kc, c * 128:(c + 1) * 128], rhs=xg.t[:, kc, 0:N],
                                                                 start=(kc == 0), stop=(kc == 1)), R=[g2t, xg], W=[ps])
                op("act", lambda e, ps=ps: e.activation(out=tC.t[:, 0:N], in_=ps.t[:, 0:N], func=AF.Copy), R=[ps], W=[tC])
                dma("sp", GG[c * 128:(c + 1) * 128, c0:c0 + N], tC.t[:, 0:N], R=[tC])
            for i_ in range(4):
                r0 = 1536 + i_ * 96
                dma("sp", th[i_].t[0:96, 0:N], RW[r0:r0 + 96, c0:c0 + N], W=[th[i_]])
                if i_ < 2:
                    op("act", lambda e, i_=i_: e.activation(out=th[i_].t[0:96, 0:N], in_=th[i_].t[0:96, 0:N], func=AF.Tanh), R=[th[i_]], W=[th[i_]])
            for b in range(N // 128):
                bs = slice(b * 128, (b + 1) * 128)
                row0 = c0 + b * 128
                for (dst, cb) in [(rtm, 0), (ktm, 4)]:
                    ps = nps()
                    for c in range(4):
                        op("pe", lambda e, ps=ps, c=c, cb=cb: e.transpose(out=ps.t[:, c * 128:(c + 1) * 128], in_=rkvc.t[:, cb + c, bs], identity=ident.t[:, :]),
                           R=[rkvc, ident], W=[ps])
                    op("act", lambda e, ps=ps, dst=dst: e.activation(out=dst.t[:, :], in_=ps.t[:, :], func=AF.Copy), R=[ps], W=[dst])
                dma("sp", TM["r"][row0:row0 + 128, :], rtm.t[:, :], R=[rtm])
                op("dve", lambda e: e.tensor_tensor(out=kkt.t[:, :], in0=ktm.t[:, :], in1=KKR.t[:, :], op=ALU.mult), R=[ktm, KKR], W=[kkt])
                op("act", lambda e: e.activation(out=tA.t[:, :], in_=kkt.t[:, :], func=AF.Square), R=[kkt], W=[tA])
                op("dve", lambda e: e.tensor_reduce(out=ss8.t[:, :], in_=tA.t[:, :].rearrange("p (h j) -> p h j", j=64), axis=AX.X, op=ALU.add),
                   R=[tA], W=[ss8])
                op("act", lambda e: e.activation(out=ss8.t[:, :], in_=ss8.t[:, :], func=AF.Sqrt, bias=eps12.t[:, 0:1]), R=[ss8, eps12], W=[ss8])
                op("dve", lambda e: e.reciprocal(out=ss8.t[:, :], in_=ss8.t[:, :]), R=[ss8], W=[ss8])
                op("dve", lambda e: e.tensor_tensor(out=kkt.t[:, :].rearrange("p (h j) -> p h j", j=64),
                                                    in0=kkt.t[:, :].rearrange("p (h j) -> p h j", j=64),
                                                    in1=ss8.t[:, :].unsqueeze(2).broadcast_to([128, 8, 64]), op=ALU.mult), R=[ss8, kkt], W=[kkt])
                dma("sp", TM["kk"][row0:row0 + 128, :], kkt.t[:, :], R=[kkt])
                for d_ in range(2):
                    ps = nps()
                    op("pe", lambda e, ps=ps, d_=d_: e.matmul(ps.t[:, :], lhsT=th[d_].t[:, bs], rhs=w2a[d_].t[:, :], start=True, stop=True),
                       R=[th[d_], w2a[d_]], W=[ps])
                    op("act", lambda e, ps=ps: e.activation(out=tB.t[:, :], in_=ps.t[:, :], func=AF.Sigmoid), R=[ps], W=[tB])
                    op("act", lambda e: e.activation(out=tB.t[:, :], in_=tB.t[:, :], func=AF.Exp, scale=-math.exp(-0.5)), R=[tB], W=[tB])
                    dma("sp", TM[f"dec{d_}"][row0:row0 + 128, :], tB.t[:, :], R=[tB])
                    ps = nps()
                    op("pe", lambda e, ps=ps, d_=d_: e.matmul(ps.t[:, :], lhsT=th[2 + d_].t[:, bs], rhs=w2a[2 + d_].t[:, :], start=True, stop=True),
                       R=[th[2 + d_], w2a[2 + d_]], W=[ps])
                    op("act", lambda e, ps=ps: e.activation(out=tC.t[:, :], in_=ps.t[:, :], func=AF.Sigmoid), R=[ps], W=[tC])
                    op("dve", lambda e: e.tensor_tensor(out=tD.t[:, :], in0=kkt.t[:, :], in1=tC.t[:, :], op=ALU.mult), R=[kkt, tC], W=[tD])
                    dma("sp", TM[f"b{d_}"][row0:row0 + 128, :], tD.t[:, :], R=[tD])
                    op("dve", lambda e: e.tensor_tensor(out=tC.t[:, :], in0=tC.t[:, :], in1=KAR.t[:, :], op=ALU.mult), R=[tC, KAR], W=[tC])
                    op("dve", lambda e: e.tensor_tensor(out=tC.t[:, :], in0=tC.t[:, :], in1=OMK.t[:, :], op=ALU.add), R=[tC, OMK], W=[tC])
                    op("dve", lambda e: e.tensor_tensor(out=tC.t[:, :], in0=tC.t[:, :], in1=ktm.t[:, :], op=ALU.mult), R=[tC, ktm], W=[tC])
                    dma("sp", TM[f"kd{d_}"][row0:row0 + 128, :], tC.t[:, :], R=[tC])
        pg.barrier()
        pg.pop()

    def rwkv_scan(l):
        pg.push()
        CVN = 128
        groups = [("p", [pcol(0), pcol(1)], SEQ, 1, [0, 1]), ("p", [pcol(2), pcol(3)], SEQ, 1, [2, 3]), ("s", [SBASE], TS, 2, None)]
        NBMAX = 2
        S = pg.buf([128, 2, 8, 64], F32, "S")
        RB = [pg.buf([128, 5, 2, 512], F32, f"RB{i}") for i in range(2)]
        tmp = pg.buf([128, 2, 8, 64], F32, "tmp")
        vk = [pg.buf([128, 2, 8, 64], F32, f"vk{i}") for i in range(2)]
        sa = pg.buf([128, 2, 8], F32, "sa")
        VSB = pg.buf([128, 2, 8, CVN], F32, "VSB")
        VTMP = pg.buf([128, 2, 8, CVN], F32, "VTMP")
        OB = pg.buf([128, 2, 8, CVN], F32, "OB")
        OTMP = pg.buf([128, 2, 8, CVN], F32, "OTMP")
        names = [("dec0", "dec1"), ("b0", "b1"), ("kd0", "kd1"), ("kk", "kk"), ("r", "r")]
        for (kind, bases, T, C, seqids) in groups[:cfg.get("scan_groups", 3)]:
            T = cfg.get("scan_T", T) if kind == "s" else T
            nb = len(bases)
            if kind == "p":
                op("dve", lambda e: e.memset(S.t[:, :, :, :], 0.0), W=[S])
            else:
                dma("sp", S.t[0:64, 0, :, :], sf_in[l, :, :, :].rearrange("h i j -> i h j"), W=[S])
                dma("sp", S.t[64:128, 0, :, :], sb_in[l, :, :, :].rearrange("h i j -> i h j"), W=[S])
            Treal = SEQ if kind == "p" else TS
            nchunk = T // C
            for ch in range(nchunk):
                t0 = ch * C
                if t0 % CVN == 0:
                    for bi, base in enumerate(bases):
                        srcf = bass.AP(tensor=VS, offset=base + t0, ap=[[NCOL, 64], [64 * NCOL, 8], [1, CVN]])
                        dma("sp", VSB.t[0:64, bi, :, :], srcf, W=[VSB])
                        srcb = bass.AP(tensor=VS, offset=base + Treal - t0 - CVN, ap=[[NCOL, 64], [64 * NCOL, 8], [1, CVN]])
                        dma("sp", VTMP.t[64:128, bi, :, :], srcb, W=[VTMP])
                    for bi, base in enumerate(bases):
                        op("pool", lambda e, bi=bi: e.tensor_copy(out=VSB.t[64:128, bi, :, :], in_=VTMP.t[64:128, bi, :, ::-1]), R=[VTMP], W=[VSB])
                rb = RB[ch % 2]
                for ai, (n0, n1) in enumerate(names):
                    for bi, base in enumerate(bases):
                        srcf = bass.AP(tensor=TM[n0], offset=(base + t0) * 512, ap=[[0, 64], [512, C], [1, 512]])
                        dstf = rb.t[0:64, ai, :, :].rearrange("p (c b) n -> p c b n", b=nb)[:, :, bi, :] if C * nb == 2 else None
                        dma("sp", dstf, srcf, W=[rb])
                        srcb = bass.AP(tensor=TM[n1], offset=(base + Treal - 1 - t0) * 512, ap=[[0, 64], [-512, C], [1, 512]])
                        dstb = rb.t[64:128, ai, :, :].rearrange("p (c b) n -> p c b n", b=nb)[:, :, bi, :]
                        dma("sp", dstb, srcb, W=[rb])
                for ci in range(C):
                    t = t0 + ci
                    col = t % CVN

                    def row(ai):
                        return rb.t[:, ai, ci * nb:(ci + 1) * nb, :].rearrange("p b (h j) -> p b h j", j=64)
                    Sv = S.t[:, 0:nb, :, :]
                    Tv = tmp.t[:, 0:nb, :, :]
                    vkb = vk[t % 2]
                    Vv = vkb.t[:, 0:nb, :, :]
                    op("pool", lambda e: e.tensor_tensor(out=Vv, in0=row(2), in1=VSB.t[:, 0:nb, :, col:col + 1].broadcast_to([128, nb, 8, 64]), op=ALU.mult),
                       R=[rb, VSB], W=[vkb])
                    op("dve", lambda e: e.tensor_tensor(out=Tv, in0=Sv, in1=row(3), op=ALU.mult), R=[S, rb], W=[tmp])
                    op("dve", lambda e: e.tensor_reduce(out=sa.t[:, 0:nb, :], in_=Tv, axis=AX.X, op=ALU.add), R=[tmp], W=[sa])
                    op("dve", lambda e: e.tensor_tensor(out=Sv, in0=Sv, in1=row(0), op=ALU.mult), R=[S, rb], W=[S])
                    op("dve", lambda e: e.tensor_tensor(out=Tv, in0=row(1), in1=sa.t[:, 0:nb, :].unsqueeze(3).broadcast_to([128, nb, 8, 64]), op=ALU.mult),
                       R=[rb, sa], W=[tmp])
                    op("dve", lambda e: e.tensor_tensor(out=Sv, in0=Sv, in1=Tv, op=ALU.subtract), R=[S, tmp], W=[S])
                    op("dve", lambda e: e.tensor_tensor(out=Sv, in0=Sv, in1=Vv, op=ALU.add), R=[S, vkb], W=[S])
                    op("dve", lambda e: e.tensor_tensor(out=Tv, in0=Sv, in1=row(4), op=ALU.mult), R=[S, rb], W=[tmp])
                    op("dve", lambda e: e.tensor_reduce(out=OB.t[:, 0:nb, :, col], in_=Tv, axis=AX.X, op=ALU.add), R=[tmp], W=[OB])
                    if col == CVN - 1:
                        tb = t - (CVN - 1)
                        for bi, base in enumerate(bases):
                            dstf = bass.AP(tensor=OS[0], offset=base + tb, ap=[[NCOL, 64], [64 * NCOL, 8], [1, CVN]])
                            dma("sp", dstf, OB.t[0:64, bi, :, :], R=[OB])
                            op("pool", lambda e, bi=bi: e.tensor_copy(out=OTMP.t[64:128, bi, :, :], in_=OB.t[64:128, bi, :, ::-1]), R=[OB], W=[OTMP])
                            dstb = bass.AP(tensor=OS[1], offset=base + Treal - tb - CVN, ap=[[NCOL, 64], [64 * NCOL, 8], [1, CVN]])
                            dma("sp", dstb, OTMP.t[64:128, bi, :, :], R=[OTMP])
            if kind == "p":
                for bi, sid in enumerate(seqids):
                    dma("sp", nsf_out[sid, l, :, :, :].rearrange("h i j -> i h j"), S.t[0:64, bi, :, :], R=[S])
                    dma("sp", nsb_out[sid, l, :, :, :].rearrange("h i j -> i h j"), S.t[64:128, bi, :, :], R=[S])
        pg.barrier()
        pg.pop()

    def rwkv_post(l):
        pg.push()
        o0 = pg.buf([128, 512], F32, "o0")
        o1 = pg.buf([128, 512], F32, "o1")
        mu = pg.buf([128, 512], F32, "mu")
        bg = pg.buf([128, 2, 512], F32, "bg")
        gne = pg.buf([128, 1], F32, "gne")
        op("dve", lambda e: e.memset(gne.t[:, :], GN_EPS), W=[gne])
        for (c0, N) in SEGS[:cfg.get("rw_segs", 12)]:
            for c in range(4):
                rows = slice(c * 128, (c + 1) * 128)
                dma("sp", o0.t[:, 0:N], OS[0][rows, c0:c0 + N], W=[o0])
                dma("sp", o1.t[:, 0:N], OS[1][rows, c0:c0 + N], W=[o1])
                dma("sp", bg.t[:, 0, 0:N], BON[rows, c0:c0 + N], W=[bg])
                dma("sp", bg.t[:, 1, 0:N], GG[rows, c0:c0 + N], W=[bg])
                op("dve", lambda e: e.tensor_tensor(out=o0.t[:, 0:N], in0=o0.t[:, 0:N], in1=o1.t[:, 0:N], op=ALU.add), R=[o0, o1], W=[o0])
                ps = nps()
                op("pe", lambda e, ps=ps: e.matmul(ps.t[:, 0:N], lhsT=bdiag.t[:, :], rhs=o0.t[:, 0:N], start=True, stop=True), R=[bdiag, o0], W=[ps])
                op("act", lambda e, ps=ps: e.activation(out=mu.t[:, 0:N], in_=ps.t[:, 0:N], func=AF.Copy, scale=-1.0 / 64), R=[ps], W=[mu])
                op("dve", lambda e: e.tensor_tensor(out=o0.t[:, 0:N], in0=o0.t[:, 0:N], in1=mu.t[:, 0:N], op=ALU.add), R=[o0, mu], W=[o0])
                op("act", lambda e: e.activation(out=o1.t[:, 0:N], in_=o0.t[:, 0:N], func=AF.Square), R=[o0], W=[o1])
                ps = nps()
                op("pe", lambda e, ps=ps: e.matmul(ps.t[:, 0:N], lhsT=bdiag.t[:, :], rhs=o1.t[:, 0:N], start=True, stop=True), R=[bdiag, o1], W=[ps])
                op("act", lambda e, ps=ps: e.activation(out=mu.t[:, 0:N], in_=ps.t[:, 0:N], func=AF.Sqrt, scale=1.0 / 64, bias=gne.t[:, 0:1]), R=[ps, gne], W=[mu])
                op("dve", lambda e: e.reciprocal(out=mu.t[:, 0:N], in_=mu.t[:, 0:N]), R=[mu], W=[mu])
                op("dve", lambda e: e.tensor_tensor(out=o0.t[:, 0:N], in0=o0.t[:, 0:N], in1=mu.t[:, 0:N], op=ALU.mult), R=[o0, mu], W=[o0])
                op("dve", lambda e, c=c: e.tensor_scalar(out=o0.t[:, 0:N], in0=o0.t[:, 0:N], scalar1=VC.t[:, 72 + c:73 + c], scalar2=VC.t[:, 76 + c:77 + c],
                                                       op0=ALU.mult, op1=ALU.add), R=[o0, VC], W=[o0])
                op("dve", lambda e: e.tensor_tensor(out=o0.t[:, 0:N], in0=o0.t[:, 0:N], in1=bg.t[:, 0, 0:N], op=ALU.add), R=[o0, bg], W=[o0])
                s_ = rot(stb, "stb")
                op("dve", lambda e, s_=s_: e.tensor_tensor(out=s_.t[:, 0:N], in0=o0.t[:, 0:N], in1=bg.t[:, 1, 0:N], op=ALU.mult), R=[o0, bg], W=[s_])
                dma("sp", OT[1024 + c * 128:1024 + (c + 1) * 128, c0:c0 + N], s_.t[:, 0:N], R=[s_])
        pg.barrier()
        pg.pop()


    def select_own():
        pg.push()
        qa = [pg.buf([128, 512], F32, f"qa{i}") for i in range(4)]
        acc = pg.buf([128, 512], F32, "acc")
        for s_ in range(NPS):
            for kc in range(16):
                dma("sp", acc.t[:, 0:256], XT[kc * 128:(kc + 1) * 128, pcol(s_):pcol(s_) + 256], W=[acc])
                dma("sp", XO[kc * 128:(kc + 1) * 128, s_ * 256:(s_ + 1) * 256], acc.t[:, 0:256], R=[acc])
        for kc in range(16):
            for hf in range(2):
                for q in range(4):
                    cq = SBASE + q * 1024 + hf * 512
                    dma("sp", qa[q].t[:, :], XT[kc * 128:(kc + 1) * 128, cq:cq + 512], W=[qa[q]])
                op("act", lambda e: e.activation(out=acc.t[:, :], in_=qa[0].t[:, :], func=AF.Copy, scale=selt.t[:, 0:1]), R=[qa[0], selt], W=[acc])
                for q in range(1, 4):
                    op("dve", lambda e, q=q: e.scalar_tensor_tensor(out=acc.t[:, :], in0=qa[q].t[:, :], scalar=selt.t[:, q:q + 1], in1=acc.t[:, :],
                                                                   op0=ALU.mult, op1=ALU.add), R=[qa[q], selt, acc], W=[acc])
                dma("sp", XO[kc * 128:(kc + 1) * 128, 1024 + hf * 512:1024 + (hf + 1) * 512], acc.t[:, :], R=[acc])
        pg.barrier()
        pg.pop()

    def moe_phase():
        work_alloc()
        pg.push()
        actT = pg.buf([128, 56, 512], BF16, "actTm")
        t1 = [pg.buf([128, 512], F32, f"m1_{i}") for i in range(2)]
        t3 = [pg.buf([128, 512], F32, f"m3_{i}") for i in range(2)]
        Gb = pg.buf([128, 512], F32, "Gb")
        rt = pg.buf([128, 16, 8], F32, "rt")
        es_ = pg.buf([8, 8, 128], F32, "esl")
        lg = pg.buf([128, 8], F32, "lg")
        l2 = pg.buf([128, 8], F32, "l2")
        mk1 = pg.buf([128, 8], F32, "mk1")
        mk2 = pg.buf([128, 8], F32, "mk2")
        gts = pg.buf([128, 8], F32, "gts")
        sc_ = pg.buf([128, 8], F32, "sc_")
        gT = pg.buf([8, 512], F32, "gT")
        ctr["m1"] = 0
        ctr["m3"] = 0
        dma("sp", rt.t[:, :, :], router_in.ap().rearrange("(a p) e -> p a e", p=128), W=[rt])
        dma("sp", es_.t[:, :, :], esel_in.ap().rearrange("k (e p) -> k e p", p=128), W=[es_])
        for ti in range(cfg.get("moe_tiles", 4)):
            j = 0 if ti < 2 else 1
            c0 = ti * 512
            N = 512
            dma("sp", xt.t[:, :, 0:N], fm(XO, 0, 16, c0, N), W=[xt])
            normmod(lambda kc: xt.t[:, kc, 0:N], N,
                    lambda kc: A2.t[:, kc, j:j + 1], lambda kc: MOD.t[:, 48 + kc, j:j + 1],
                    lambda kc: (xt.t[:, kc, 0:N], [xt]))
            for kc in range(16):
                op("act", lambda e, kc=kc: e.activation(out=hT.t[:, kc, 0:N], in_=xt.t[:, kc, 0:N], func=AF.Copy), R=[xt], W=[hT])
            for b in range(4):
                ps = nps()
                for kc in range(16):
                    op("pe", lambda e, ps=ps, kc=kc, b=b: e.matmul(ps.t[:, 0:8], lhsT=xt.t[:, kc, b * 128:(b + 1) * 128], rhs=rt.t[:, kc, :],
                                                                 start=(kc == 0), stop=(kc == 15)), R=[xt, rt], W=[ps])
                op("act", lambda e, ps=ps: e.activation(out=lg.t[:, :], in_=ps.t[:, 0:8], func=AF.Copy), R=[ps], W=[lg])
                op("dve", lambda e: e.tensor_reduce(out=sc_.t[:, 0:1], in_=lg.t[:, :], axis=AX.X, op=ALU.max), R=[lg], W=[sc_])
                op("dve", lambda e: e.tensor_scalar(out=mk1.t[:, :], in0=lg.t[:, :], scalar1=sc_.t[:, 0:1], scalar2=None, op0=ALU.is_equal), R=[lg, sc_], W=[mk1])
                op("dve", lambda e: e.scalar_tensor_tensor(out=l2.t[:, :], in0=mk1.t[:, :], scalar=-1e30, in1=lg.t[:, :], op0=ALU.mult, op1=ALU.add),
                   R=[mk1, lg], W=[l2])
                op("dve", lambda e: e.tensor_reduce(out=sc_.t[:, 1:2], in_=l2.t[:, :], axis=AX.X, op=ALU.max), R=[l2], W=[sc_])
                op("dve", lambda e: e.tensor_scalar(out=mk2.t[:, :], in0=l2.t[:, :], scalar1=sc_.t[:, 1:2], scalar2=None, op0=ALU.is_equal), R=[l2, sc_], W=[mk2])
                op("dve", lambda e: e.tensor_tensor(out=sc_.t[:, 2:3], in0=sc_.t[:, 1:2], in1=sc_.t[:, 0:1], op=ALU.subtract), R=[sc_], W=[sc_])
                op("act", lambda e: e.activation(out=sc_.t[:, 3:4], in_=sc_.t[:, 2:3], func=AF.Exp), R=[sc_], W=[sc_])
                op("dve", lambda e: e.tensor_scalar(out=sc_.t[:, 4:5], in0=sc_.t[:, 3:4], scalar1=1.0, scalar2=None, op0=ALU.add), R=[sc_], W=[sc_])
                op("dve", lambda e: e.reciprocal(out=sc_.t[:, 4:5], in_=sc_.t[:, 4:5]), R=[sc_], W=[sc_])
                op("dve", lambda e: e.tensor_tensor(out=sc_.t[:, 5:6], in0=sc_.t[:, 3:4], in1=sc_.t[:, 4:5], op=ALU.mult), R=[sc_], W=[sc_])
                op("dve", lambda e: e.tensor_scalar(out=gts.t[:, :], in0=mk1.t[:, :], scalar1=sc_.t[:, 4:5], scalar2=None, op0=ALU.mult), R=[mk1, sc_], W=[gts])
                op("dve", lambda e: e.scalar_tensor_tensor(out=gts.t[:, :], in0=mk2.t[:, :], scalar=sc_.t[:, 5:6], in1=gts.t[:, :], op0=ALU.mult, op1=ALU.add),
                   R=[mk2, sc_, gts], W=[gts])
                op("pe", lambda e, b=b: e.transpose(out=pss.t[0:8, b * 128:(b + 1) * 128], in_=gts.t[:, :], identity=ident.t[:, :]), R=[gts, ident], W=[pss])
            op("act", lambda e: e.activation(out=gT.t[:, :], in_=pss.t[0:8, 0:512], func=AF.Copy), R=[pss], W=[gT])
            epi = resid_epi(80, j, [c0], XT=XO)
            for ex in range(cfg.get("moe_experts", 8)):
                ps = nps()
                op("pe", lambda e, ps=ps, ex=ex: e.matmul(ps.t[:, :], lhsT=es_.t[:, ex, :], rhs=gT.t[:, :], start=True, stop=True), R=[es_, gT], W=[ps])
                op("act", lambda e, ps=ps: e.activation(out=Gb.t[:, :], in_=ps.t[:, :], func=AF.Copy), R=[ps], W=[Gb])
                for fi in range(D_FFE // 512):
                    w1 = rot(wt, "wt")
                    dma("pool", w1.t[:, :, :], W_m1[ex // 2][(ex % 2) * D:(ex % 2 + 1) * D, fi * 512:(fi + 1) * 512].rearrange("(a p) n -> p a n", p=128), W=[w1])
                    w3 = rot(wt, "wt")
                    dma("pool", w3.t[:, :, :], W_m3[ex // 2][(ex % 2) * D:(ex % 2 + 1) * D, fi * 512:(fi + 1) * 512].rearrange("(a p) n -> p a n", p=128), W=[w3])
                    for nc_ in range(4):
                        p1 = nps()
                        p3 = nps()
                        for (pp, ww) in [(p1, w1), (p3, w3)]:
                            for kc in range(16):
                                op("pe", lambda e, pp=pp, ww=ww, kc=kc, nc_=nc_: e.matmul(
                                    pp.t[:, :], lhsT=ww.t[:, kc, nc_ * 128:(nc_ + 1) * 128], rhs=hT.t[:, kc, 0:N],
                                    start=(kc == 0), stop=(kc == 15)), R=[ww, hT], W=[pp])
                        a1 = rot(t1, "m1")
                        a3 = rot(t3, "m3")
                        op("act", lambda e, a1=a1, p1=p1: e.activation(out=a1.t[:, :], in_=p1.t[:, :], func=AF.Silu), R=[p1], W=[a1])
                        op("act", lambda e, a3=a3, p3=p3: e.activation(out=a3.t[:, :], in_=p3.t[:, :], func=AF.Copy), R=[p3], W=[a3])
                        op("dve", lambda e, a1=a1, a3=a3: e.tensor_tensor(out=a1.t[:, :], in0=a1.t[:, :], in1=a3.t[:, :], op=ALU.mult), R=[a1, a3], W=[a1])
                        fc = fi * 4 + nc_
                        op("dve", lambda e, a1=a1, fc=fc: e.tensor_tensor(out=actT.t[:, fc, :], in0=a1.t[:, :], in1=Gb.t[:, :], op=ALU.mult), R=[a1, Gb], W=[actT])
                for n_ in range(16):
                    w2 = rot(wt, "wt")
                    w2v = w2.t[:, :, :].rearrange("p a b -> p (a b)")[:, 0:56 * 128].rearrange("p (k n) -> p k n", n=128)
                    dma("pool", w2v, W_m2[ex // 2][(ex % 2) * D_FFE:(ex % 2 + 1) * D_FFE, n_ * 128:(n_ + 1) * 128].rearrange("(k p) n -> p k n", p=128), W=[w2])
                    ps = nps()
                    for kc in range(56):
                        op("pe", lambda e, ps=ps, w2v=w2v, kc=kc: e.matmul(ps.t[:, :], lhsT=w2v[:, kc, :], rhs=actT.t[:, kc, :],
                                                                         start=(kc == 0), stop=(kc == 55)), R=[w2, actT], W=[ps])
                    epi(ps, n_, 0, N)
        pg.barrier()
        pg.pop()
        work_free()

    def final_phase(src, ncols_tiles):
        work_alloc()
        for (T_, c0, N, outT, orow) in src:
            dma("sp", xt.t[:, :, 0:N], fm(T_, 0, 16, c0, N), W=[xt])
            normmod(lambda kc: xt.t[:, kc, 0:N], N, lambda kc: FING.t[:, kc:kc + 1], None, lambda kc: (xt.t[:, kc, 0:N], [xt]))
            for b in range(N // 128):
                for g in range(4):
                    ps = nps()
                    for jj in range(4):
                        kc = g * 4 + jj
                        op("pe", lambda e, ps=ps, jj=jj, kc=kc, b=b: e.transpose(out=ps.t[:, jj * 128:(jj + 1) * 128], in_=xt.t[:, kc, b * 128:(b + 1) * 128],
                                                                                identity=ident.t[:, :]), R=[xt, ident], W=[ps])
                    s_ = rot(stf, "stf")
                    op("act", lambda e, ps=ps, s_=s_: e.activation(out=s_.t[:, :], in_=ps.t[:, :], func=AF.Copy), R=[ps], W=[s_])
                    dma("sp", outT[orow + b * 128:orow + (b + 1) * 128, g * 512:(g + 1) * 512], s_.t[:, :], R=[s_])
        work_free()

    if cfg.get("test_tail"):
        compute_mods(1)
        select_own()
        moe_phase()
        final_phase([(XO, i * 512, 512, yp_out if i < 2 else ys_out, (i % 2) * 512) for i in range(4)], None)
        pg.barrier()
        pg.replay()
        return
    for l in range(cfg.get("layers", DEPTH)):
        compute_mods(l)
        if cfg.get("stop_after") == "M":
            break
        phaseA(l)
        if cfg.get("stop_after") == "A":
            break
        if not cfg.get("skip_att"):
            attention(l)
        if cfg.get("stop_after") == "T":
            break
        if not cfg.get("skip_pool"):
            pool_phase(l)
        if cfg.get("stop_after") == "P":
            break
        rwkv_prep(l)
        if cfg.get("stop_after") == "R1":
            break
        rwkv_scan(l)
        if cfg.get("stop_after") == "R2":
            break
        rwkv_post(l)
        if cfg.get("stop_after") == "R3":
            break
        wout_phase(l)
        if cfg.get("stop_after") == "W":
            break
        if l == 0:
            ffn_phase(l)
        if cfg.get("stop_after") == "F":
            break
        if l == 1:
            select_own()
            moe_phase()
            final_phase([(XO, i * 512, 512, yp_out if i < 2 else ys_out, (i % 2) * 512) for i in range(4)], None)

    pg.barrier()
    pg.replay()


ALL_BIGS = ["ada_w0", "w_in0", "w_out0", "ffn_w1", "ffn_w3", "ffn_w2", "ada_w1", "w_in1", "w_out1",
            ] + [f"moe_w{k}_{i}" for i in range(4) for k in (1, 3, 2)]
CFG = {"bigs": ALL_BIGS, "layers": 2}


def _consts():
    ident = np.eye(128, dtype=np.float32)
    col = np.arange(64)
    cs = np.clip(col - 8, 0, 48)
    valid = (col[None, :] >= cs[:, None]) & (col[None, :] < cs[:, None] + 16)
    mT = np.where(valid.T, 0.0, -30000.0).astype(np.float32)
    namask = np.concatenate([mT, mT], axis=0)
    invcnt = np.zeros((2, 4, TS), np.float32)
    for si, T in enumerate([SEQ, TS]):
        t = np.arange(T)
        for gi, win in enumerate((2, 4, 8, 16)):
            lo = np.maximum(t - win // 2, 0)
            hi = np.minimum(t + win - win // 2, T)
            invcnt[si, gi, :T] = 1.0 / (hi - lo)
    esel = np.zeros((8, 8, 128), np.float32)
    for e in range(8):
        esel[e, e, :] = 1.0
    return ident, namask, invcnt, esel.reshape(8, 8 * 128).copy()


def kernel(**inp):
    cfg = CFG
    f = lambda a: np.ascontiguousarray(np.asarray(a, dtype=np.float32))
    ident, namask, invcnt, esel = _consts()
    L = DEPTH
    vecs = np.zeros((L, 84, 128), np.float32)
    rowv = np.zeros((L, 8, 512), np.float32)
    for l in range(L):
        vecs[l, 0:16] = f(inp["norm1_g"][l]).reshape(16, 128)
        vecs[l, 16:32] = f(inp["norm2_g"][l]).reshape(16, 128)
        vecs[l, 32:68] = f(inp["rw_conv"][l]).reshape(36, 128)
        vecs[l, 68:72] = f(inp["rw_rk"][l]).reshape(4, 128)
        vecs[l, 72:76] = f(inp["rw_ln_g"][l]).reshape(4, 128)
        vecs[l, 76:80] = f(inp["rw_ln_b"][l]).reshape(4, 128)
        vecs[l, 80:84] = f(inp["pool_scale"][l]).reshape(4, 128)
        rowv[l, 0:2] = f(inp["rw_w0"][l])
        rowv[l, 2:4] = f(inp["rw_a0"][l])
        rowv[l, 4] = f(inp["rw_kk"][l])
        rowv[l, 5] = f(inp["rw_ka"][l])
        rowv[l, 6] = f(inp["rw_rk"][l])
    bigsrc = {}
    for l in range(L):
        bigsrc[f"w_in{l}"] = f(inp["w_in"][l])
        bigsrc[f"w_out{l}"] = f(inp["w_out"][l])
        bigsrc[f"ada_w{l}"] = f(inp["ada_w"][l])
    bigsrc["ffn_w1"] = f(inp["ffn_w1"][0])
    bigsrc["ffn_w3"] = f(inp["ffn_w3"][0])
    bigsrc["ffn_w2"] = f(inp["ffn_w2"][0])
    for nm, key, rr, cc in [("moe_w1", "moe_w1", D, D_FFE), ("moe_w3", "moe_w3", D, D_FFE), ("moe_w2", "moe_w2", D_FFE, D)]:
        a = f(inp[key][0])
        for i in range(4):
            bigsrc[f"{nm}_{i}"] = a[2 * i:2 * i + 2].reshape(2 * rr, cc)
    common = {
        "ident": ident, "namask": namask, "invcnt": invcnt, "esel": esel, "vecs": vecs,
        "ada_b": f(inp["ada_b"]).reshape(L, 96, 128), "final_g": f(inp["final_g"]).reshape(16, 128),
        "na_rpb": f(inp["na_rpb"]).reshape(L, 240, 31), "rowv": rowv,
        "rw_w2": f(inp["rw_w2"]), "rw_a2": f(inp["rw_a2"]), "rw_g2": f(inp["rw_g2"]),
        "pool_w": f(inp["pool_w"]), "router": f(inp["moe_router"][0]),
    }
    in_maps = []
    for c in range(8):
        b = c // 4
        m = dict(common)
        m["x_prompt"] = f(inp["x_prompt"][4 * c:4 * c + 4]).reshape(NPS * SEQ, D)
        m["x_sample"] = f(inp["x_sample"][b])
        m["cache_k"] = f(inp["cache_attn_k"][b]).reshape(L, 256, 1024)
        m["cache_v"] = f(inp["cache_attn_v"][b]).reshape(L, 256, 1024)
        m["state_f"] = f(inp["state_rwkv_fwd"][b])
        m["state_b"] = f(inp["state_rwkv_bwd"][b])
        m["cond"] = np.stack([f(inp["c_ctx"]), f(inp["c"][b])], axis=0)
        sel = np.zeros((128, 4), np.float32)
        sel[:, c % 4] = 1.0
        m["sel"] = sel
        for n in cfg["bigs"]:
            a = bigsrc[n]
            r = a.shape[0] // 8
            m[n] = a[c * r:(c + 1) * r]
        in_maps.append(m)
    if cfg.get("nogather"):
        for m in in_maps:
            for n in cfg["bigs"]:
                del m[n]
                m[n + "_fullin"] = bigsrc[n]
    kernel.in_maps = in_maps
    if cfg.get("dry"):
        return None
    nc = build(cfg)
    ncores = cfg.get("ncores", 8)
    res = run_bass_kernel_spmd(nc, in_maps[:ncores], core_ids=list(range(ncores)))
    R = list(res.results) + [res.results[0]] * (8 - ncores)
    y_prompt = np.concatenate([R[c]["y_prompt"].reshape(NPS, SEQ, D) for c in range(8)], axis=0)
    y_sample = np.stack([np.concatenate([R[b * 4 + q]["y_sample"] for q in range(4)], axis=0) for b in range(2)], axis=0)
    nk = np.concatenate([R[c]["new_k"] for c in range(8)], axis=0).reshape(32, L, SEQ, 16, 64)
    nv = np.concatenate([R[c]["new_v"] for c in range(8)], axis=0).reshape(32, L, SEQ, 16, 64)
    nsf = np.concatenate([R[c]["new_sf"] for c in range(8)], axis=0)
    nsb = np.concatenate([R[c]["new_sb"] for c in range(8)], axis=0)
    kernel.last = R
    return (y_prompt, y_sample, nk, nv, nsf, nsb)
```
